# Optimizing a Trainium2 kernel written in Bass

```python
import math
import jax, jax.numpy as jnp
from jax import lax
import numpy as np

D_MODEL = 2048
BATCH = 2
SEQ = 4096
DEPTH = 1

HEAD_DIM = 128
N_HEADS = D_MODEL // HEAD_DIM
N_DIL_HEADS = N_HEADS // 2
N_FOX_HEADS = N_HEADS - N_DIL_HEADS
DIL_WIDTH = N_DIL_HEADS * HEAD_DIM
FOX_WIDTH = N_FOX_HEADS * HEAD_DIM
DIL_PATTERNS = ((128, 1), (512, 4), (2048, 16))
Q_BLOCK = 128
IN_SPLITS = (DIL_WIDTH, 2 * DIL_WIDTH, 3 * DIL_WIDTH,
             3 * DIL_WIDTH + FOX_WIDTH, 3 * DIL_WIDTH + 2 * FOX_WIDTH,
             3 * DIL_WIDTH + 3 * FOX_WIDTH)
IN_WIDTH = 3 * DIL_WIDTH + 3 * FOX_WIDTH + N_FOX_HEADS
N_EXPERTS = 32
TOP_K = 4
D_FF = D_MODEL
SWIGLU_LIMIT = 7.0
SWIGLU_ALPHA = 1.702
EXPERT_BLOCK = 128
LN_EPS = 1e-5
DEEPNORM_ALPHA = (2.0 * DEPTH) ** 0.25
DEEPNORM_BETA = (8.0 * DEPTH) ** -0.25

kernel_name = 'hybrid_dilated_fox_moe_block'


def layer_norm(x, g, b):
    xf = x.astype(jnp.float32)
    mu = jnp.mean(xf, axis=-1, keepdims=True)
    xc = xf - mu
    var = jnp.mean(xc * xc, axis=-1, keepdims=True)
    return (xc * lax.rsqrt(var + LN_EPS) * g.astype(jnp.float32) + b.astype(jnp.float32)).astype(x.dtype)


def alibi_slopes(n):
    return 2.0 ** (-8.0 * jnp.arange(1, n + 1, dtype=jnp.float32) / n)


def dilated_window_partial(q, k, v, slopes, window, dilation):
    B, S, H, Dh = q.shape
    L = S // dilation
    nb = -(-L // Q_BLOCK)
    Lp = nb * Q_BLOCK
    reach = window // dilation

    def to_sub(t):
        t = t.reshape(B, L, dilation, H, Dh).transpose(0, 2, 3, 1, 4)
        t = jnp.pad(t, ((0, 0), (0, 0), (0, 0), (0, Lp - L), (0, 0)))
        return t.reshape(B, dilation, H, nb, Q_BLOCK, Dh)

    def with_prev(t):
        prev = jnp.pad(t, ((0, 0), (0, 0), (0, 0), (1, 0), (0, 0), (0, 0)))[:, :, :, :-1]
        return jnp.concatenate([prev, t], axis=4)

    qs = to_sub(q)
    kk = with_prev(to_sub(k))
    vv = with_prev(to_sub(v))
    scores = jnp.einsum('brhnqe,brhnke->brhnqk', qs, kk).astype(jnp.float32) / math.sqrt(Dh)
    qi = jnp.arange(Q_BLOCK)[:, None]
    kj = jnp.arange(2 * Q_BLOCK)[None, :]
    delta = qi + Q_BLOCK - kj
    kpos = (jnp.arange(nb)[:, None, None] - 1) * Q_BLOCK + kj[None]
    valid = (delta >= 0) & (delta <= reach) & (kpos >= 0)
    bias = -slopes[:, None, None, None] * (delta * dilation).astype(jnp.float32)
    scores = jnp.where(valid, scores + bias, -jnp.inf)
    m = jnp.max(scores, axis=-1, keepdims=True)
    p = jnp.exp(scores - m)
    l = jnp.sum(p, axis=-1)
    o = jnp.einsum('brhnqk,brhnke->brhnqe', p, vv.astype(jnp.float32))
    o = o.reshape(B, dilation, H, Lp, Dh)[:, :, :, :L].transpose(0, 3, 1, 2, 4).reshape(B, S, H, Dh)
    m = m.reshape(B, dilation, H, Lp)[:, :, :, :L].transpose(0, 3, 1, 2).reshape(B, S, H)
    l = l.reshape(B, dilation, H, Lp)[:, :, :, :L].transpose(0, 3, 1, 2).reshape(B, S, H)
    return o, m, l


def dilated_attention(q, k, v, slopes):
    parts = [dilated_window_partial(q, k, v, slopes, w, d) for (w, d) in DIL_PATTERNS]
    m_max = jnp.max(jnp.stack([p[1] for p in parts]), axis=0)
    num = 0.0
    den = 0.0
    for o_i, m_i, l_i in parts:
        wgt = jnp.exp(m_i - m_max)
        num = num + wgt[..., None] * o_i
        den = den + wgt * l_i
    return num / den[..., None]


def forgetting_attention(q, k, v, log_f):
    B, S, H, Dh = q.shape
    cum = jnp.cumsum(log_f, axis=1).transpose(0, 2, 1)
    kt = k.transpose(0, 2, 1, 3)
    vt = v.transpose(0, 2, 1, 3).astype(jnp.float32)
    nq = S // Q_BLOCK
    qb = q.reshape(B, nq, Q_BLOCK, H, Dh).transpose(1, 0, 3, 2, 4)
    cb = cum.reshape(B, H, nq, Q_BLOCK).transpose(2, 0, 1, 3)
    starts = jnp.arange(nq) * Q_BLOCK
    kpos = jnp.arange(S)
    scale = 1.0 / math.sqrt(Dh)

    def block(args):
        qi, ci, s0 = args
        s = jnp.einsum('bhqe,bhke->bhqk', qi, kt).astype(jnp.float32) * scale
        s = s + ci[..., None] - cum[:, :, None, :]
        qpos = s0 + jnp.arange(Q_BLOCK)
        s = jnp.where(kpos[None, :] <= qpos[:, None], s, -jnp.inf)
        p = jax.nn.softmax(s, axis=-1)
        return jnp.einsum('bhqk,bhke->bhqe', p, vt)

    o = lax.map(block, (qb, cb, starts))
    return o.transpose(1, 0, 3, 2, 4).reshape(B, S, H, Dh)


def moe_ffn(h, w_router, b_router, w_gate_up, b_gate_up, w_down, b_down):
    B, S, D = h.shape
    hf = h.reshape(-1, D)
    T = hf.shape[0]
    logits = (hf @ w_router + b_router).astype(jnp.float32)
    top_val, top_idx = lax.top_k(logits, TOP_K)
    gates = jax.nn.softmax(top_val, axis=-1)
    TK = T * TOP_K
    e_flat = top_idx.reshape(-1)
    g_flat = gates.reshape(-1)
    tok_flat = jnp.arange(TK, dtype=jnp.int32) // TOP_K
    counts = jnp.bincount(e_flat, length=N_EXPERTS)
    padded = (counts + EXPERT_BLOCK - 1) // EXPERT_BLOCK * EXPERT_BLOCK
    pad_end = jnp.cumsum(padded)
    pad_start = pad_end - padded
    grp_start = jnp.cumsum(counts) - counts
    order = jnp.argsort(e_flat)
    e_sorted = e_flat[order]
    dest = pad_start[e_sorted] + jnp.arange(TK) - grp_start[e_sorted]
    nblk = (TK + N_EXPERTS * (EXPERT_BLOCK - 1) + EXPERT_BLOCK - 1) // EXPERT_BLOCK
    row_tok = jnp.zeros((nblk * EXPERT_BLOCK,), jnp.int32).at[dest].set(tok_flat[order])
    row_gate = jnp.zeros((nblk * EXPERT_BLOCK,), jnp.float32).at[dest].set(g_flat[order])
    blk_exp = jnp.minimum(jnp.searchsorted(pad_end, jnp.arange(nblk) * EXPERT_BLOCK, side='right'),
                          N_EXPERTS - 1)

    def expert_block(args):
        tok, gate, e = args
        xb = hf[tok]
        gu = xb @ w_gate_up[e] + b_gate_up[e]
        g, u = jnp.split(gu, 2, axis=-1)
        g = jnp.minimum(g, SWIGLU_LIMIT)
        u = jnp.clip(u, -SWIGLU_LIMIT, SWIGLU_LIMIT)
        act = (u + 1.0) * (g * jax.nn.sigmoid(SWIGLU_ALPHA * g))
        y = act @ w_down[e] + b_down[e]
        return y.astype(jnp.float32) * gate[:, None]

    y = lax.map(expert_block, (row_tok.reshape(nblk, EXPERT_BLOCK),
                               row_gate.reshape(nblk, EXPERT_BLOCK), blk_exp))
    out = jnp.zeros((T, D), jnp.float32).at[row_tok].add(y.reshape(-1, D))
    return out.reshape(B, S, D).astype(h.dtype)


def setup_inputs(seed: int = 0) -> dict:
    key = jax.random.key(seed)
    ks = jax.random.split(key, 17)
    f32 = jnp.float32
    D, E, F = D_MODEL, N_EXPERTS, D_FF
    nrm = lambda k, shape, s: jax.random.normal(k, shape, f32) * s
    return {
        'x': nrm(ks[0], (BATCH, SEQ, D), 1.0),
        'c': nrm(ks[1], (BATCH, D), 1.0),
        'w_ada': nrm(ks[2], (DEPTH, D, 6 * D), D ** -0.5),
        'b_ada': nrm(ks[3], (DEPTH, 6 * D), 0.02),
        'w_in': nrm(ks[4], (DEPTH, D, IN_WIDTH), D ** -0.5),
        'b_forget': 3.0 + nrm(ks[5], (DEPTH, N_FOX_HEADS), 0.1),
        'w_out': nrm(ks[6], (DEPTH, D, D), D ** -0.5 * DEEPNORM_BETA),
        'ln1_g': 1.0 + nrm(ks[7], (DEPTH, D), 0.02),
        'ln1_b': nrm(ks[8], (DEPTH, D), 0.02),
        'w_router': nrm(ks[9], (DEPTH, D, E), D ** -0.5),
        'b_router': nrm(ks[10], (DEPTH, E), 0.01),
        'w_gate_up': nrm(ks[11], (DEPTH, E, D, 2 * F), D ** -0.5),
        'b_gate_up': nrm(ks[12], (DEPTH, E, 2 * F), 0.02),
        'w_down': nrm(ks[13], (DEPTH, E, F, D), F ** -0.5 * DEEPNORM_BETA),
        'b_down': nrm(ks[14], (DEPTH, E, D), 0.02),
        'ln2_g': 1.0 + nrm(ks[15], (DEPTH, D), 0.02),
        'ln2_b': nrm(ks[16], (DEPTH, D), 0.02),
    }


def reference(x, c, w_ada, b_ada, w_in, b_forget, w_out, ln1_g, ln1_b, w_router, b_router,
              w_gate_up, b_gate_up, w_down, b_down, ln2_g, ln2_b):
    B, S, D = x.shape
    slopes = alibi_slopes(N_DIL_HEADS)
    cond = jax.nn.silu(c)
    heads = lambda t: t.reshape(B, S, -1, HEAD_DIM)
    for layer in range(DEPTH):
        mod = cond @ w_ada[layer] + b_ada[layer]
        sh_a, sc_a, g_a, sh_m, sc_m, g_m = jnp.split(mod[:, None, :], 6, axis=-1)
        h = x * (1.0 + sc_a) + sh_a
        proj = h @ w_in[layer]
        qa, ka, va, qf, kf, vf, f_logit = jnp.split(proj, IN_SPLITS, axis=-1)
        o_dil = dilated_attention(heads(qa), heads(ka), heads(va), slopes)
        log_f = jax.nn.log_sigmoid((f_logit + b_forget[layer]).astype(jnp.float32))
        o_fox = forgetting_attention(heads(qf), heads(kf), heads(vf), log_f)
        mixed = jnp.concatenate([o_dil.reshape(B, S, DIL_WIDTH), o_fox.reshape(B, S, FOX_WIDTH)],
                                axis=-1).astype(x.dtype)
        attn = mixed @ w_out[layer]
        x = layer_norm(DEEPNORM_ALPHA * x + g_a * attn, ln1_g[layer], ln1_b[layer])
        h = x * (1.0 + sc_m) + sh_m
        ffn = moe_ffn(h, w_router[layer], b_router[layer], w_gate_up[layer], b_gate_up[layer],
                      w_down[layer], b_down[layer])
        x = layer_norm(DEEPNORM_ALPHA * x + g_m * ffn, ln2_g[layer], ln2_b[layer])
    return x
```

```python
import math
import numpy as np
import concourse.bass as bass
import concourse.mybir as mybir
from concourse.bass_utils import run_bass_kernel_spmd

F32 = mybir.dt.float32
BF16 = mybir.dt.bfloat16
AF = mybir.ActivationFunctionType
ALU = mybir.AluOpType
AX = mybir.AxisListType

D = 2048
NCH = 16
S_LOC = 4096
NT_OWN = 8
E = 32
E_RUN = 32
NEG = -30000.0
SQ = math.sqrt(128.0)
ISQ = 1.0 / SQ
ALPHA = 2.0 ** 0.25
EPS = 1e-5
DBG = {}
STOP = None


class _Stop(Exception):
    pass


class Eng:
    def __init__(self, nc, h, name, nring=0):
        self.nc = nc
        self.h = h
        self.name = name
        self.sem = nc.semaphore(name + "_s").__enter__()
        self.n = 0
        self.seen = {}
        self.ring = [nc.semaphore(f"{name}_r{i}").__enter__() for i in range(nring)]
        self.rcnt = [0] * nring
        self.rnext = 0

    def wait(self, *toks):
        for t in toks:
            if t is None:
                continue
            sem, v, key = t
            if self.seen.get(key, 0) >= v:
                continue
            self.h.wait_ge(sem, v)
            self.seen[key] = v

    def sig(self, ins):
        self.n += 1
        ins.then_inc(self.sem, 1)
        return (self.sem, self.n, self.name)

    def dma_sig(self, ins):
        i = self.rnext
        self.rnext = (self.rnext + 1) % len(self.ring)
        self.rcnt[i] += 1
        ins.then_inc(self.ring[i], 16)
        return (self.ring[i], 16 * self.rcnt[i], f"{self.name}_r{i}")


class Buf:
    def __init__(self):
        self.w = None
        self.r = {}


def _deps(eng, reads, writes):
    for b in reads:
        eng.wait(b.w)
    for b in writes:
        eng.wait(b.w, *b.r.values())


def _commit(tok, reads, writes):
    for b in reads:
        old = b.r.get(tok[2])
        if old is None or old[1] < tok[1]:
            b.r[tok[2]] = tok
    for b in writes:
        b.w = tok
        b.r = {}


def OP(eng, emit, reads=(), writes=()):
    _deps(eng, reads, writes)
    ins = emit()
    tok = eng.sig(ins)
    _commit(tok, reads, writes)
    return tok


def DMA(eng, out, in_, reads=(), writes=()):
    _deps(eng, reads, writes)
    i = eng.rnext
    if eng.rcnt[i]:
        eng.wait((eng.ring[i], 16 * eng.rcnt[i], f"{eng.name}_r{i}"))
    ins = eng.h.dma_start(out=out, in_=in_)
    tok = eng.dma_sig(ins)
    _commit(tok, reads, writes)
    return tok


def build_nc():
    nc = bass.Bass("TRN2", target_bir_lowering=False)

    def din(name, shape, dt=F32):
        return nc.dram_tensor(name, list(shape), dt, kind="ExternalInput").ap()

    xs = din("xs", [S_LOC, D])
    kval = din("kval", [1, S_LOC])
    cb = din("cb", [128, NCH])
    w_ada = din("w_ada", [D, 6 * D])
    b_ada_c = din("b_ada_c", [128, 96])
    w_in = din("w_in", [D, 6152])
    bf_c = din("bf_c", [8, 1])
    w_out = din("w_out", [D, D])
    ln1_g = din("ln1_g", [1, D])
    ln1_b = din("ln1_b", [1, D])
    w_router = din("w_router", [D, E])
    b_router = din("b_router", [1, E])
    w_gu = din("w_gu", [E_RUN, D, 2 * D])
    b_gu_c = din("b_gu_c", [128, E, 32])
    w_dn = din("w_dn", [E_RUN, D, D])
    b_dn = din("b_dn", [E_RUN, D])
    ln2_g = din("ln2_g", [1, D])
    ln2_b = din("ln2_b", [1, D])
    ident_d = din("ident", [128, 128])
    dmult_d = din("dmult", [128, 17 * 128])
    ddelta_d = din("ddelta", [128, 17 * 128])
    cm4_d = din("cm4", [128, 512])
    y = nc.dram_tensor("y", [NT_OWN * 128, D], F32, kind="ExternalOutput").ap()

    KT_scr = nc.dram_tensor("KT_scr", [16, 128, S_LOC], BF16, kind="Internal").ap()
    QT_scr = nc.dram_tensor("QT_scr", [16, 128, 1024], BF16, kind="Internal").ap()
    V_scr = nc.dram_tensor("V_scr", [8, 128, 32, 256], BF16, kind="Internal").ap()

    pe = Eng(nc, nc.tensor, "pe")
    act = Eng(nc, nc.scalar, "act")
    dve = Eng(nc, nc.vector, "dve")
    pool = Eng(nc, nc.gpsimd, "pool", nring=8)
    sp = Eng(nc, nc.sync, "sp", nring=8)

    from contextlib import ExitStack
    glob = ExitStack()

    def barrier():
        toks = [(e.sem, e.n, e.name) for e in (pe, act, dve) if e.n > 0]
        for q in (sp, pool):
            for i, sem in enumerate(q.ring):
                if q.rcnt[i]:
                    toks.append((sem, 16 * q.rcnt[i], f"{q.name}_r{i}"))
        for e in (pe, act, dve, pool, sp):
            e.wait(*toks)

    def sb(stack, name, shape, dt=F32):
        return stack.enter_context(nc.sbuf_tensor(name, list(shape), dt))

    PS = [glob.enter_context(nc.psum_tensor(f"ps{i}", [128, 512], F32)) for i in range(8)]
    PSB = [Buf() for _ in range(8)]

    ident_f = sb(glob, "ident_f", [128, 128]); B_ident_f = Buf()
    ident_b = sb(glob, "ident_b", [128, 128], BF16); B_ident_b = Buf()
    ones_f = sb(glob, "ones_f", [128, 128]); B_ones_f = Buf()
    ones_b = sb(glob, "ones_b", [128, 128], BF16); B_ones_b = Buf()
    modc = sb(glob, "modc", [128, 96]); B_modc = Buf()
    sc1 = sb(glob, "sc1", [128, 32]); B_sc1 = Buf()
    cbt = sb(glob, "cbt", [128, NCH]); B_cbt = Buf()
    cond_b = sb(glob, "cond_b", [128, NCH], BF16); B_cond = Buf()
    badac = sb(glob, "badac", [128, 96]); B_badac = Buf()
    eps_c = sb(glob, "eps_c", [128, 1]); B_epsc = Buf()
    ab = ExitStack()
    nchi = sb(ab, "nchi", [8, S_LOC], BF16)
    ncmid = sb(ab, "ncmid", [8, S_LOC], BF16)
    nclo = sb(ab, "nclo", [8, S_LOC], BF16)
    B_ncs = Buf()
    sel = sb(ab, "sel", [8, 8, 128], BF16); B_sel = Buf()
    kval_b = sb(ab, "kval_b", [1, S_LOC], BF16); B_kvalb = Buf()
    MT_scr = nc.dram_tensor("MT_scr", [128, NCH, 1024], BF16, kind="Internal").ap()

    DMA(sp, ident_f[:], ident_d[:, :], writes=[B_ident_f])
    DMA(sp, cbt[:], cb[:, :], writes=[B_cbt])
    DMA(sp, badac[:], b_ada_c[:, :], writes=[B_badac])
    OP(dve, lambda: nc.vector.tensor_copy(out=ident_b[:], in_=ident_f[:]), reads=[B_ident_f], writes=[B_ident_b])
    OP(dve, lambda: nc.vector.memset(eps_c[:], EPS), writes=[B_epsc])
    OP(dve, lambda: nc.vector.memset(ones_f[:], 1.0), writes=[B_ones_f])
    OP(dve, lambda: nc.vector.memset(ones_b[:], 1.0), writes=[B_ones_b])
    for h in range(8):
        OP(dve, lambda h=h: nc.vector.tensor_copy(out=sel[:, h, :], in_=ident_f[0:8, h:h + 1].to_broadcast([8, 128])),
           reads=[B_ident_f], writes=[B_sel])
    OP(act, lambda: nc.scalar.activation(out=cond_b[:], in_=cbt[:], func=AF.Silu), reads=[B_cbt], writes=[B_cond])

    try:
        with ExitStack() as st:
            wa = [sb(st, f"wa{i}", [128, NCH, 512], BF16) for i in range(2)]
            B_wa = [Buf() for _ in range(2)]
            w_ada_v = w_ada.rearrange("(k p) n -> p k n", p=128)
            NB = 24
            DMA(pool, wa[0][:], w_ada_v[:, :, 0:512], writes=[B_wa[0]])
            for blk in range(NB):
                if blk + 1 < NB:
                    s = (blk + 1) % 2
                    DMA(pool, wa[s][:], w_ada_v[:, :, (blk + 1) * 512:(blk + 2) * 512], writes=[B_wa[s]])
                s = blk % 2

                def emit(blk=blk, s=s):
                    ins = None
                    for nchk in range(4):
                        col = blk * 4 + nchk
                        for k in range(NCH):
                            ins = nc.tensor.matmul(PS[0][:, col:col + 1], lhsT=wa[s][:, k, nchk * 128:(nchk + 1) * 128],
                                                   rhs=cond_b[:, k:k + 1], start=(k == 0), stop=(k == NCH - 1))
                    return ins
                OP(pe, emit, reads=[B_wa[s], B_cond], writes=[PSB[0]])
            OP(dve, lambda: nc.vector.tensor_tensor(out=modc[:], in0=PS[0][:, 0:96], in1=badac[:], op=ALU.add),
               reads=[PSB[0], B_badac], writes=[B_modc])
            OP(dve, lambda: nc.vector.tensor_scalar(out=sc1[:, 0:16], in0=modc[:, 16:32], scalar1=1.0, scalar2=None, op0=ALU.add),
               reads=[B_modc], writes=[B_sc1])
            OP(dve, lambda: nc.vector.tensor_scalar(out=sc1[:, 16:32], in0=modc[:, 64:80], scalar1=1.0, scalar2=None, op0=ALU.add),
               reads=[B_modc], writes=[B_sc1])
        if "modc" in DBG:
            DMA(sp, DBG["modc"][:, :], modc[:], reads=[B_modc])
        barrier()
        if STOP == "p0":
            raise _Stop()

        w_in_v = w_in.rearrange("(k p) n -> p k n", p=128)
        with ExitStack() as st:
            hT = sb(st, "hT", [128, NCH, 2048], BF16); B_hT = Buf()
            xt = [sb(st, f"xt{i}", [128, D]) for i in range(2)]; B_xt = [Buf() for _ in range(2)]
            wr_ = [sb(st, f"wi{i}", [128, NCH, 256], BF16) for i in range(3)]; B_wr = [Buf() for _ in range(3)]
            kst = [sb(st, f"kst{i}", [128, 2048], BF16) for i in range(2)]; B_kst = [Buf() for _ in range(2)]
            qst = [sb(st, f"qst{i}", [128, 512], BF16) for i in range(2)]; B_qst = [Buf() for _ in range(2)]
            vst = [sb(st, f"vst{i}", [128, 16, 256], BF16) for i in range(2)]; B_vst = [Buf() for _ in range(2)]
            wf = sb(st, "wf", [128, NCH, 8], BF16); B_wf = Buf()
            bfc = sb(st, "bfc", [8, 1]); B_bfc = Buf()
            ftmp = [sb(st, f"ftmp{i}", [8, 512]) for i in range(4)]; B_ft = [Buf() for _ in range(4)]
            kval8 = sb(st, "kval8", [8, S_LOC]); B_kval8 = Buf()
            DMA(sp, kval8[:], kval[0].partition_broadcast(8), writes=[B_kval8])
            OP(dve, lambda: nc.vector.tensor_copy(out=kval_b[:], in_=kval8[0:1, :]), reads=[B_kval8], writes=[B_kvalb])
            rawc = [sb(st, f"rawc{i}", [8, 512]) for i in range(2)]; B_rawc = [Buf() for _ in range(2)]
            ones8 = sb(st, "ones8", [8, 512]); B_ones8 = Buf()
            one_c = sb(st, "one_c", [8, 1]); B_onec = Buf()
            OP(dve, lambda: nc.vector.memset(ones8[:], 1.0), writes=[B_ones8])
            OP(dve, lambda: nc.vector.memset(one_c[:], 1.0), writes=[B_onec])
            DMA(sp, bfc[:], bf_c[:, :], writes=[B_bfc])
            with nc.allow_non_contiguous_dma(reason="tiny forget-gate weight slice"):
                DMA(pool, wf[:], w_in_v[:, :, 6144:6152], writes=[B_wf])

            blocks = []
            for g, (qo, ko, vo, hbase) in enumerate([(0, 1024, 2048, 0), (3072, 4096, 5120, 8)]):
                for hp in range(4):
                    blocks.append(("q", hbase + 2 * hp, qo + hp * 256))
                    blocks.append(("k", hbase + 2 * hp, ko + hp * 256))
                    blocks.append(("v", g * 4 + hp, vo + hp * 256))
            psrot = [0]

            def next_ps(lo=0, n=4):
                i = lo + psrot[0] % n
                psrot[0] += 1
                return i

            cnt = {"k": 0, "q": 0, "v": 0, "w": 0, "scan": 0}
            for half in range(2):
                for tt in range(16):
                    s = tt % 2
                    DMA(sp, xt[s][:], xs[(half * 16 + tt) * 128:(half * 16 + tt + 1) * 128, :], writes=[B_xt[s]])
                    for q4 in range(4):
                        pi = next_ps(0, 4)

                        def emit(q4=q4, s=s, pi=pi):
                            ins = None
                            for cc in range(4):
                                c = q4 * 4 + cc
                                ins = nc.tensor.transpose(out=PS[pi][:, cc * 128:(cc + 1) * 128], in_=xt[s][:, c * 128:(c + 1) * 128],
                                                          identity=ident_f[:])
                            return ins
                        OP(pe, emit, reads=[B_xt[s], B_ident_f], writes=[PSB[pi]])
                        for cc in range(4):
                            c = q4 * 4 + cc
                            if cc % 2 == 0:
                                OP(act, lambda c=c, cc=cc, pi=pi, tt=tt: nc.scalar.activation(
                                    out=hT[:, c, tt * 128:(tt + 1) * 128], in_=PS[pi][:, cc * 128:(cc + 1) * 128],
                                    func=AF.Identity, bias=modc[:, c:c + 1], scale=sc1[:, c:c + 1]),
                                   reads=[PSB[pi], B_modc, B_sc1], writes=[B_hT])
                            else:
                                OP(dve, lambda c=c, cc=cc, pi=pi, tt=tt: nc.vector.tensor_scalar(
                                    out=hT[:, c, tt * 128:(tt + 1) * 128], in0=PS[pi][:, cc * 128:(cc + 1) * 128],
                                    scalar1=sc1[:, c:c + 1], scalar2=modc[:, c:c + 1], op0=ALU.mult, op1=ALU.add),
                                   reads=[PSB[pi], B_modc, B_sc1], writes=[B_hT])
                nblk = len(blocks)

                def load_w(bi):
                    s = cnt["w"] % 3
                    cnt["w"] += 1
                    col = blocks[bi][2]
                    DMA(pool, wr_[s][:], w_in_v[:, :, col:col + 256], writes=[B_wr[s]])
                    return s
                slots = {}
                slots[0] = load_w(0)
                slots[1] = load_w(1)
                for bi in range(nblk):
                    if bi + 2 < nblk:
                        slots[bi + 2] = load_w(bi + 2)
                    kind, hidx, col = blocks[bi]
                    ws = slots[bi]
                    if kind == "k":
                        for hh in range(2):
                            ks = cnt["k"] % 2
                            cnt["k"] += 1
                            for tc in range(4):
                                pi = next_ps(0, 4)

                                def emit(hh=hh, tc=tc, pi=pi, ws=ws):
                                    ins = None
                                    for c in range(NCH):
                                        ins = nc.tensor.matmul(PS[pi][:, :], lhsT=wr_[ws][:, c, hh * 128:(hh + 1) * 128],
                                                               rhs=hT[:, c, tc * 512:(tc + 1) * 512], start=(c == 0), stop=(c == NCH - 1))
                                    return ins
                                OP(pe, emit, reads=[B_wr[ws], B_hT], writes=[PSB[pi]])
                                if tc % 2 == 0:
                                    OP(act, lambda ks=ks, tc=tc, pi=pi: nc.scalar.copy(out=kst[ks][:, tc * 512:(tc + 1) * 512], in_=PS[pi][:, :]),
                                       reads=[PSB[pi]], writes=[B_kst[ks]])
                                else:
                                    OP(dve, lambda ks=ks, tc=tc, pi=pi: nc.vector.tensor_copy(out=kst[ks][:, tc * 512:(tc + 1) * 512], in_=PS[pi][:, :]),
                                       reads=[PSB[pi]], writes=[B_kst[ks]])
                            DMA(sp, KT_scr[hidx + hh, :, half * 2048:(half + 1) * 2048], kst[ks][:], reads=[B_kst[ks]])
                    elif kind == "q":
                        for hh in range(2):
                            qs = cnt["q"] % 2
                            cnt["q"] += 1
                            pi = next_ps(0, 4)
                            hT4 = hT[:, :, :].rearrange("p c (b t) -> p c b t", t=128)

                            def emit(hh=hh, pi=pi, ws=ws):
                                ins = None
                                for kk in range(4):
                                    for c in range(NCH):
                                        ins = nc.tensor.matmul(PS[pi][:, kk * 128:(kk + 1) * 128], lhsT=wr_[ws][:, c, hh * 128:(hh + 1) * 128],
                                                               rhs=hT[:, c, (4 * kk + 3) * 128:(4 * kk + 4) * 128], start=(c == 0), stop=(c == NCH - 1))
                                return ins
                            OP(pe, emit, reads=[B_wr[ws], B_hT], writes=[PSB[pi]])
                            OP(act, lambda qs=qs, pi=pi: nc.scalar.copy(out=qst[qs][:, :], in_=PS[pi][:, :]), reads=[PSB[pi]], writes=[B_qst[qs]])
                            DMA(sp, QT_scr[hidx + hh, :, half * 512:(half + 1) * 512], qst[qs][:], reads=[B_qst[qs]])
                    else:
                        vs = cnt["v"] % 2
                        cnt["v"] += 1
                        for tp in range(8):
                            pi = next_ps(0, 4)

                            def emit(tp=tp, pi=pi, ws=ws):
                                ins = None
                                for t2 in range(2):
                                    tt = tp * 2 + t2
                                    for c in range(NCH):
                                        ins = nc.tensor.matmul(PS[pi][:, t2 * 256:(t2 + 1) * 256], lhsT=hT[:, c, tt * 128:(tt + 1) * 128],
                                                               rhs=wr_[ws][:, c, :], start=(c == 0), stop=(c == NCH - 1))
                                return ins
                            OP(pe, emit, reads=[B_wr[ws], B_hT], writes=[PSB[pi]])
                            if tp % 2 == 0:
                                OP(act, lambda vs=vs, tp=tp, pi=pi: nc.scalar.copy(
                                    out=vst[vs][:, 2 * tp:2 * tp + 2, :], in_=PS[pi][:, :].rearrange("p (a b) -> p a b", a=2)),
                                   reads=[PSB[pi]], writes=[B_vst[vs]])
                            else:
                                OP(dve, lambda vs=vs, tp=tp, pi=pi: nc.vector.tensor_copy(
                                    out=vst[vs][:, 2 * tp:2 * tp + 2, :], in_=PS[pi][:, :].rearrange("p (a b) -> p a b", a=2)),
                                   reads=[PSB[pi]], writes=[B_vst[vs]])
                        DMA(sp, V_scr[hidx, :, half * 16:(half + 1) * 16, :], vst[vs][:], reads=[B_vst[vs]])
                for tc in range(4):
                    pi = 4 + (tc % 2)
                    gc0 = half * 2048 + tc * 512

                    def emit(tc=tc, pi=pi):
                        ins = None
                        for c in range(NCH):
                            ins = nc.tensor.matmul(PS[pi][0:8, :], lhsT=wf[:, c, 0:8], rhs=hT[:, c, tc * 512:(tc + 1) * 512],
                                                   start=(c == 0), stop=(c == NCH - 1))
                        return ins
                    OP(pe, emit, reads=[B_wf, B_hT], writes=[PSB[pi]])
                    yv, av, lv, rv = ftmp
                    By, Ba, Bl, Br = B_ft
                    ev, Be, t5, B5 = av, Ba, rv, Br
                    OP(dve, lambda pi=pi: nc.vector.tensor_scalar(out=yv[:], in0=PS[pi][0:8, :], scalar1=bfc[:, 0:1], scalar2=None, op0=ALU.add),
                       reads=[PSB[pi], B_bfc], writes=[By])
                    OP(act, lambda: nc.scalar.activation(out=av[:], in_=yv[:], func=AF.Abs), reads=[By], writes=[Ba])
                    OP(act, lambda: nc.scalar.activation(out=ev[:], in_=av[:], func=AF.Exp, scale=-1.0), reads=[Ba], writes=[Be])
                    OP(act, lambda: nc.scalar.activation(out=lv[:], in_=ev[:], func=AF.Ln, bias=one_c[:, 0:1], scale=1.0),
                       reads=[Be, B_onec], writes=[Bl])
                    OP(dve, lambda: nc.vector.tensor_scalar(out=rv[:], in0=yv[:], scalar1=0.0, scalar2=None, op0=ALU.min), reads=[By], writes=[Br])
                    OP(dve, lambda: nc.vector.tensor_tensor(out=lv[:], in0=lv[:], in1=rv[:], op=ALU.subtract), reads=[Bl, Br], writes=[Bl])
                    sidx = cnt["scan"]
                    cnt["scan"] += 1
                    cur, prev = rawc[sidx % 2], rawc[(sidx + 1) % 2]
                    Bcur, Bprev = B_rawc[sidx % 2], B_rawc[(sidx + 1) % 2]
                    if sidx == 0:
                        OP(dve, lambda cur=cur: nc.vector.tensor_tensor_scan(out=cur[:], data0=ones8[:], data1=lv[:], initial=0.0,
                                                                             op0=ALU.mult, op1=ALU.add),
                           reads=[Bl, B_ones8], writes=[Bcur])
                    else:
                        OP(dve, lambda cur=cur, prev=prev: nc.vector.tensor_tensor_scan(out=cur[:], data0=ones8[:], data1=lv[:],
                                                                                        initial=prev[:, 511:512], op0=ALU.mult, op1=ALU.add),
                           reads=[Bl, B_ones8, Bprev], writes=[Bcur])
                    OP(dve, lambda cur=cur, gc0=gc0: nc.vector.scalar_tensor_tensor(out=t5[:], in0=cur[:], scalar=SQ, in1=kval8[:, gc0:gc0 + 512],
                                                                                     op0=ALU.mult, op1=ALU.add),
                       reads=[Bcur, B_kval8], writes=[B5])
                    OP(dve, lambda gc0=gc0: nc.vector.tensor_copy(out=nchi[:, gc0:gc0 + 512], in_=t5[:]), reads=[B5], writes=[B_ncs])
                    OP(dve, lambda gc0=gc0: nc.vector.tensor_tensor(out=t5[:], in0=t5[:], in1=nchi[:, gc0:gc0 + 512], op=ALU.subtract),
                       reads=[B5, B_ncs], writes=[B5])
                    OP(dve, lambda gc0=gc0: nc.vector.tensor_copy(out=ncmid[:, gc0:gc0 + 512], in_=t5[:]), reads=[B5], writes=[B_ncs])
                    OP(dve, lambda gc0=gc0: nc.vector.tensor_tensor(out=t5[:], in0=t5[:], in1=ncmid[:, gc0:gc0 + 512], op=ALU.subtract),
                       reads=[B5, B_ncs], writes=[B5])
                    OP(dve, lambda gc0=gc0: nc.vector.tensor_copy(out=nclo[:, gc0:gc0 + 512], in_=t5[:]), reads=[B5], writes=[B_ncs])

        barrier()
        if STOP == "A":
            raise _Stop()
        with ExitStack() as st:
            KT = [sb(st, f"KT{i}", [128, S_LOC], BF16) for i in range(2)]; B_KT = [Buf() for _ in range(2)]
            Vh = [sb(st, f"Vh{i}", [128, 32, 128], BF16) for i in range(2)]; B_Vh = [Buf() for _ in range(2)]
            QT = [sb(st, f"QT{i}", [128, 1024], BF16) for i in range(2)]; B_QT = [Buf() for _ in range(2)]
            T0 = sb(st, "T0", [128, 2176]); B_T0 = Buf()
            DS = sb(st, "DS", [128, 2176]); B_DS = Buf()
            Bt = sb(st, "Bt", [128, 2176]); B_Bt = Buf()
            Bhi = sb(st, "Bhi", [128, 2176], BF16); B_Bhi = Buf()
            Blo = sb(st, "Blo", [128, 2176], BF16); B_Blo = Buf()
            cm4 = sb(st, "cm4_sb", [128, 512], BF16); B_cm4 = Buf()
            cm4f = sb(st, "cm4f", [128, 512]); B_cm4f = Buf()
            Pm = [sb(st, f"Pm{i}", [128, S_LOC], BF16) for i in range(2)]; B_Pm = [Buf() for _ in range(2)]
            PT = [sb(st, f"PT{i}", [128, 4, 128], BF16) for i in range(4)]; B_PT = [Buf() for _ in range(4)]
            mx = [sb(st, f"mx{i}", [128, 16]) for i in range(2)]; B_mx = [Buf() for _ in range(2)]
            rs = [sb(st, f"rs{i}", [128, 16]) for i in range(2)]; B_rs = [Buf() for _ in range(2)]
            osb = [sb(st, f"osb{i}", [128, 128], BF16) for i in range(2)]; B_osb = [Buf() for _ in range(2)]
            mixT = sb(st, "mixT", [128, NCH, 1024], BF16); B_mixT = [Buf() for _ in range(NCH)]

            DMA(sp, T0[:], dmult_d[:, :], writes=[B_T0])
            DMA(sp, DS[:], ddelta_d[:, :], writes=[B_DS])
            DMA(sp, cm4f[:], cm4_d[:, :], writes=[B_cm4f])
            OP(dve, lambda: nc.vector.tensor_copy(out=cm4[:], in_=cm4f[:]), reads=[B_cm4f], writes=[B_cm4])
            OP(dve, lambda: nc.vector.tensor_scalar(out=Bt[:], in0=T0[:], scalar1=0.5, scalar2=NEG, op0=ALU.is_lt, op1=ALU.mult),
               reads=[B_T0], writes=[B_Bt])
            OP(dve, lambda: nc.vector.tensor_scalar(out=T0[:], in0=T0[:], scalar1=1.0, scalar2=None, op0=ALU.max), reads=[B_T0], writes=[B_T0])
            OP(act, lambda: nc.scalar.activation(out=T0[:], in_=T0[:], func=AF.Ln), reads=[B_T0], writes=[B_T0])
            OP(dve, lambda: nc.vector.scalar_tensor_tensor(out=T0[:], in0=T0[:], scalar=SQ, in1=Bt[:], op0=ALU.mult, op1=ALU.add),
               reads=[B_T0, B_Bt], writes=[B_T0])
            OP(dve, lambda: nc.vector.tensor_scalar(out=DS[:], in0=DS[:], scalar1=SQ, scalar2=None, op0=ALU.mult), reads=[B_DS], writes=[B_DS])

            def load_head(hd):
                s = hd % 2
                DMA(sp, KT[s][:], KT_scr[hd, :, :], writes=[B_KT[s]])
                DMA(sp, QT[s][:], QT_scr[hd, :, :], writes=[B_QT[s]])
                with nc.allow_non_contiguous_dma(reason="per-head V slice, 256B rows"):
                    DMA(sp, Vh[s][:], V_scr[hd // 2, :, :, (hd % 2) * 128:(hd % 2 + 1) * 128], writes=[B_Vh[s]])

            load_head(0)
            qcnt = [0]
            ptc = [0]
            for hd in range(16):
                if hd + 1 < 16:
                    load_head(hd + 1)
                s = hd % 2
                is_fox = hd >= 8
                if not is_fox:
                    slope = 2.0 ** (-(hd + 1))
                    OP(dve, lambda slope=slope: nc.vector.scalar_tensor_tensor(out=Bt[:], in0=DS[:], scalar=-slope, in1=T0[:], op0=ALU.mult, op1=ALU.add),
                       reads=[B_DS, B_T0], writes=[B_Bt])
                    OP(dve, lambda: nc.vector.tensor_copy(out=Bhi[:], in_=Bt[:]), reads=[B_Bt], writes=[B_Bhi])
                    OP(dve, lambda: nc.vector.tensor_tensor(out=Blo[:], in0=Bt[:], in1=Bhi[:], op=ALU.subtract), reads=[B_Bt, B_Bhi], writes=[B_Blo])
                for k in range(NT_OWN):
                    qi = qcnt[0]
                    qcnt[0] += 1
                    q2 = qi % 2
                    qb = 4 * k + 3
                    if is_fox:
                        chunks = [(4 * c, 4, None) for c in range(k + 1)]
                    else:
                        chunks = [(4 * c, 4, 128 * (13 - 4 * (k - c))) for c in range(max(0, k - 3), k + 1)]
                        if k >= 4:
                            chunks.append((qb - 16, 1, 0))
                    nchk = len(chunks)

                    def s_emit(pi, ci):
                        kb0, nb, tcol = chunks[ci]
                        W = nb * 128
                        k0 = kb0 * 128

                        def emit():
                            nc.tensor.matmul(PS[pi][:, 0:W], lhsT=QT[s][:, k * 128:(k + 1) * 128], rhs=KT[s][:, k0:k0 + W], start=True, stop=False)
                            if is_fox:
                                hf = hd - 8
                                nc.tensor.matmul(PS[pi][:, 0:W], lhsT=sel[:, hf, :], rhs=nchi[:, k0:k0 + W], start=False, stop=False)
                                nc.tensor.matmul(PS[pi][:, 0:W], lhsT=sel[:, hf, :], rhs=ncmid[:, k0:k0 + W], start=False, stop=False)
                                last = (kb0 + nb - 1 != qb)
                                ins = nc.tensor.matmul(PS[pi][:, 0:W], lhsT=sel[:, hf, :], rhs=nclo[:, k0:k0 + W], start=False, stop=last)
                                if not last:
                                    ins = nc.tensor.matmul(PS[pi][:, 0:W], lhsT=ident_b[:], rhs=cm4[:], start=False, stop=True)
                            else:
                                nc.tensor.matmul(PS[pi][:, 0:W], lhsT=ident_b[:], rhs=Bhi[:, tcol:tcol + W], start=False, stop=False)
                                nc.tensor.matmul(PS[pi][:, 0:W], lhsT=ident_b[:], rhs=Blo[:, tcol:tcol + W], start=False, stop=False)
                                ins = nc.tensor.matmul(PS[pi][:, 0:W], lhsT=ones_b[0:1, :], rhs=kval_b[0:1, k0:k0 + W], start=False, stop=True)
                            return ins
                        rd = [B_QT[s], B_KT[s]] + ([B_sel, B_ncs, B_ident_b, B_cm4] if is_fox else [B_ident_b, B_Bhi, B_Blo, B_ones_b, B_kvalb])
                        OP(pe, emit, reads=rd, writes=[PSB[pi]])
                        return W

                    for ci in range(nchk):
                        pi = next_ps(0, 2)
                        W = s_emit(pi, ci)
                        OP(dve, lambda pi=pi, ci=ci, W=W: nc.vector.reduce_max(out=mx[q2][:, ci:ci + 1], in_=PS[pi][:, 0:W], axis=AX.X),
                           reads=[PSB[pi]], writes=[B_mx[q2]])
                    OP(dve, lambda: nc.vector.reduce_max(out=mx[q2][:, 15:16], in_=mx[q2][:, 0:nchk], axis=AX.X), reads=[B_mx[q2]], writes=[B_mx[q2]])
                    OP(dve, lambda: nc.vector.tensor_scalar(out=mx[q2][:, 14:15], in0=mx[q2][:, 15:16], scalar1=-ISQ, scalar2=None, op0=ALU.mult),
                       reads=[B_mx[q2]], writes=[B_mx[q2]])
                    pcol = []
                    off = 0
                    for ci in range(nchk):
                        pi = next_ps(0, 2)
                        W = s_emit(pi, ci)
                        OP(act, lambda pi=pi, ci=ci, W=W, off=off: nc.scalar.activation(
                            out=Pm[q2][:, off:off + W], in_=PS[pi][:, 0:W], func=AF.Exp, bias=mx[q2][:, 14:15], scale=ISQ,
                            accum_out=rs[q2][:, ci:ci + 1]),
                           reads=[PSB[pi], B_mx[q2]], writes=[B_Pm[q2], B_rs[q2]])
                        pcol.append(off)
                        off += W
                    OP(dve, lambda: nc.vector.reduce_sum(out=rs[q2][:, 15:16], in_=rs[q2][:, 0:nchk], axis=AX.X), reads=[B_rs[q2]], writes=[B_rs[q2]])
                    OP(dve, lambda: nc.vector.reciprocal(out=rs[q2][:, 14:15], in_=rs[q2][:, 15:16]), reads=[B_rs[q2]], writes=[B_rs[q2]])
                    groups = []
                    for ci in range(nchk):
                        kb0, nb, _ = chunks[ci]
                        groups.append((kb0, nb, pcol[ci]))
                    po = 4
                    ngr = len(groups)
                    for gi, (kb0, nb, pc0) in enumerate(groups):
                        pti = ptc[0] % 4
                        ptc[0] += 1
                        pib = 5 + (pti % 2)
                        psv = PS[pib][:, :].bitcast(BF16)

                        def emit_t(nb=nb, pc0=pc0, psv=psv):
                            ins = None
                            for bb in range(nb):
                                ins = nc.tensor.transpose(out=psv[:, bb * 128:(bb + 1) * 128], in_=Pm[q2][:, pc0 + bb * 128:pc0 + (bb + 1) * 128],
                                                          identity=ident_b[:])
                            return ins
                        OP(pe, emit_t, reads=[B_Pm[q2], B_ident_b], writes=[PSB[pib]])
                        if gi % 2 == 0:
                            OP(dve, lambda nb=nb, pti=pti, psv=psv: nc.vector.tensor_copy(
                                out=PT[pti][:, 0:nb, :], in_=psv[:, 0:nb * 128].rearrange("p (a b) -> p a b", b=128)),
                               reads=[PSB[pib]], writes=[B_PT[pti]])
                        else:
                            OP(act, lambda nb=nb, pti=pti, psv=psv: nc.scalar.copy(
                                out=PT[pti][:, 0:nb, :], in_=psv[:, 0:nb * 128].rearrange("p (a b) -> p a b", b=128)),
                               reads=[PSB[pib]], writes=[B_PT[pti]])

                        def emit_pv(nb=nb, kb0=kb0, pti=pti, gi=gi):
                            ins = None
                            for bb in range(nb):
                                ins = nc.tensor.matmul(PS[po][:, 0:128], lhsT=PT[pti][:, bb, :], rhs=Vh[s][:, kb0 + bb, :],
                                                       start=(gi == 0 and bb == 0), stop=(gi == ngr - 1 and bb == nb - 1))
                            return ins
                        OP(pe, emit_pv, reads=[B_PT[pti], B_Vh[s]], writes=[PSB[po]])
                    OP(act, lambda: nc.scalar.activation(out=osb[q2][:], in_=PS[po][:, 0:128], func=AF.Copy, scale=rs[q2][:, 14:15]),
                       reads=[PSB[po], B_rs[q2]], writes=[B_osb[q2]])
                    psv7 = PS[7][:, :].bitcast(BF16)
                    OP(pe, lambda psv7=psv7: nc.tensor.transpose(out=psv7[:, 0:128], in_=osb[q2][:], identity=ident_b[:]),
                       reads=[B_osb[q2], B_ident_b], writes=[PSB[7]])
                    OP(dve, lambda psv7=psv7: nc.vector.tensor_copy(out=mixT[:, hd, k * 128:(k + 1) * 128], in_=psv7[:, 0:128]),
                       reads=[PSB[7]], writes=[B_mixT[hd]])
            B_MT = Buf()
            DMA(sp, MT_scr[:, :, :], mixT[:], reads=B_mixT, writes=[B_MT])
            if "mixT" in DBG:
                DMA(sp, DBG["mixT"].rearrange("p (c t) -> p c t", c=NCH), mixT[:], reads=B_mixT)
        ab.close()
        barrier()
        if STOP == "B":
            raise _Stop()

        with ExitStack() as st:
            acc = sb(st, "acc", [128, NT_OWN, D]); B_acc = [Buf() for _ in range(NT_OWN)]
            Gm = sb(st, "Gm", [128, D]); B_Gm = Buf()
            dg = [sb(st, f"dg{i}", [128, 128]) for i in range(2)]; B_dg = [Buf() for _ in range(2)]
            tmpc = sb(st, "tmpc", [128, 512]); B_tmpc = Buf()
            stats = sb(st, "stats", [128, 4, 6]); B_stats = Buf()
            mv = sb(st, "mv", [128, 4]); B_mv = Buf()

            def make_G(G, B_G, col0):
                for c in range(NCH):
                    d2 = c % 2
                    OP(dve, lambda c=c, d2=d2: nc.vector.tensor_scalar(out=dg[d2][:], in0=ident_f[:], scalar1=modc[:, col0 + c:col0 + c + 1],
                                                                         scalar2=None, op0=ALU.mult),
                       reads=[B_ident_f, B_modc], writes=[B_dg[d2]])
                    pi = 6 + (c % 2)
                    OP(pe, lambda d2=d2, pi=pi: nc.tensor.matmul(PS[pi][:, 0:128], lhsT=ones_f[:], rhs=dg[d2][:], start=True, stop=True),
                       reads=[B_ones_f, B_dg[d2]], writes=[PSB[pi]])
                    OP(dve, lambda c=c, pi=pi: nc.vector.tensor_copy(out=G[:, c * 128:(c + 1) * 128], in_=PS[pi][:, 0:128]),
                       reads=[PSB[pi]], writes=[B_G])

            def ln_inplace(ap, B_ap, gt, B_gt, bt, B_bt, dst=None, B_dst=None):
                if dst is None:
                    dst, B_dst = ap, B_ap
                for q in range(4):
                    OP(dve, lambda q=q: nc.vector.bn_stats(out=stats[:, q, :], in_=ap[:, q * 512:(q + 1) * 512]), reads=[B_ap], writes=[B_stats])
                OP(dve, lambda: nc.vector.bn_aggr(out=mv[:, 0:2], in_=stats[:, :, :].rearrange("p a b -> p (a b)")), reads=[B_stats], writes=[B_mv])
                OP(act, lambda: nc.scalar.activation(out=mv[:, 3:4], in_=mv[:, 1:2], func=AF.Sqrt, bias=eps_c[:, 0:1], scale=1.0),
                   reads=[B_mv, B_epsc], writes=[B_mv])
                OP(dve, lambda: nc.vector.reciprocal(out=mv[:, 2:3], in_=mv[:, 3:4]), reads=[B_mv], writes=[B_mv])
                OP(dve, lambda: nc.vector.tensor_scalar(out=dst, in0=ap, scalar1=mv[:, 0:1], scalar2=mv[:, 2:3], op0=ALU.subtract, op1=ALU.mult),
                   reads=[B_ap, B_mv], writes=[B_dst])
                OP(dve, lambda: nc.vector.tensor_tensor(out=dst, in0=dst, in1=gt[:], op=ALU.mult), reads=[B_dst, B_gt], writes=[B_dst])
                OP(dve, lambda: nc.vector.tensor_tensor(out=dst, in0=dst, in1=bt[:], op=ALU.add), reads=[B_dst, B_bt], writes=[B_dst])

            make_G(Gm, B_Gm, 80)
            with ExitStack() as st2:
                wo = sb(st2, "wo", [128, NCH, D], BF16); B_wo = Buf()
                Ga = sb(st2, "Ga", [128, D]); B_Ga = Buf()
                l1g = sb(st2, "l1g", [128, D]); B_l1g = Buf()
                l1b = sb(st2, "l1b", [128, D]); B_l1b = Buf()
                xo = sb(st2, "xo", [128, D]); B_xo = Buf()
                mixk = [sb(st2, f"mixk{i}", [128, NCH, 128], BF16) for i in range(2)]; B_mixk = [Buf() for _ in range(2)]
                w_out_v = w_out.rearrange("(k p) n -> p k n", p=128)
                for q in range(4):
                    DMA(pool, wo[:, :, q * 512:(q + 1) * 512], w_out_v[:, :, q * 512:(q + 1) * 512], writes=[B_wo])
                DMA(sp, l1g[:], ln1_g[0].partition_broadcast(128), writes=[B_l1g])
                DMA(sp, l1b[:], ln1_b[0].partition_broadcast(128), writes=[B_l1b])
                make_G(Ga, B_Ga, 32)
                for k in range(NT_OWN):
                    mk = k % 2
                    with nc.allow_non_contiguous_dma(reason="mixed tile reload, 256B rows"):
                        DMA(sp, mixk[mk][:], MT_scr[:, :, k * 128:(k + 1) * 128], reads=[B_MT], writes=[B_mixk[mk]])
                    DMA(sp, xo[:], xs[(4 * k + 3) * 128:(4 * k + 4) * 128, :], writes=[B_xo])
                    for nt in range(4):
                        pi = nt

                        def emit(nt=nt, pi=pi, mk=mk):
                            ins = None
                            for c in range(NCH):
                                ins = nc.tensor.matmul(PS[pi][:, :], lhsT=mixk[mk][:, c, :], rhs=wo[:, c, nt * 512:(nt + 1) * 512],
                                                       start=(c == 0), stop=(c == NCH - 1))
                            return ins
                        OP(pe, emit, reads=[B_mixk[mk], B_wo], writes=[PSB[pi]])
                        OP(dve, lambda nt=nt, pi=pi: nc.vector.tensor_tensor(out=tmpc[:], in0=PS[pi][:, :], in1=Ga[:, nt * 512:(nt + 1) * 512], op=ALU.mult),
                           reads=[PSB[pi], B_Ga], writes=[B_tmpc])
                        OP(dve, lambda nt=nt, k=k: nc.vector.scalar_tensor_tensor(out=acc[:, k, nt * 512:(nt + 1) * 512], in0=xo[:, nt * 512:(nt + 1) * 512],
                                                                                   scalar=ALPHA, in1=tmpc[:], op0=ALU.mult, op1=ALU.add),
                           reads=[B_xo, B_tmpc], writes=[B_acc[k]])
                    ln_inplace(acc[:, k, :], B_acc[k], l1g, B_l1g, l1b, B_l1b)
                    if "x1" in DBG:
                        DMA(sp, DBG["x1"][k * 128:(k + 1) * 128, :], acc[:, k, :], reads=[B_acc[k]])

            barrier()
            if STOP == "C1":
                raise _Stop()
            h2T = sb(st, "h2T", [128, NCH, 1024], BF16); B_h2T = Buf()
            gates = sb(st, "gates", [128, NT_OWN, E]); B_gates = Buf()
            bguc = sb(st, "bguc", [128, E, 32]); B_bguc = Buf()
            DMA(sp, bguc[:], b_gu_c[:, :, :], writes=[B_bguc])
            with ExitStack() as st2:
                h2f = sb(st2, "h2f", [128, NCH, 128]); B_h2f = Buf()
                wrt = sb(st2, "wrt", [128, NCH, E]); B_wrt = Buf()
                brt = sb(st2, "brt", [128, E]); B_brt = Buf()
                lg = sb(st2, "lg", [128, E]); B_lg = Buf()
                m8 = sb(st2, "m8", [128, 8]); B_m8 = Buf()
                eg = sb(st2, "eg", [128, E]); B_eg = Buf()
                msk = sb(st2, "msk", [128, E]); B_msk = Buf()
                sm = sb(st2, "sm", [128, 4]); B_sm = Buf()
                gT = sb(st2, "gT", [E, 128], BF16); B_gT = Buf()
                bdf = sb(st2, "bdf", [E_RUN, D]); B_bdf = Buf()
                bdb = sb(st2, "bdb", [E_RUN, D], BF16); B_bdb = Buf()
                with nc.allow_non_contiguous_dma(reason="router weights, 128B rows"):
                    DMA(sp, wrt[:], w_router.rearrange("(k p) e -> p k e", p=128), writes=[B_wrt])
                DMA(sp, brt[:], b_router[0].partition_broadcast(128), writes=[B_brt])
                DMA(sp, bdf[:], b_dn[:, :], writes=[B_bdf])
                OP(dve, lambda: nc.vector.tensor_copy(out=bdb[:], in_=bdf[:]), reads=[B_bdf], writes=[B_bdb])
                for k in range(NT_OWN):
                    x1 = acc[:, k, :]
                    B_x1 = B_acc[k]
                    for q4 in range(4):
                        pi = 4 + (q4 % 2)

                        def emit(q4=q4, pi=pi, x1=x1):
                            ins = None
                            for cc in range(4):
                                c = q4 * 4 + cc
                                ins = nc.tensor.transpose(out=PS[pi][:, cc * 128:(cc + 1) * 128], in_=x1[:, c * 128:(c + 1) * 128], identity=ident_f[:])
                            return ins
                        OP(pe, emit, reads=[B_x1, B_ident_f], writes=[PSB[pi]])
                        for cc in range(4):
                            c = q4 * 4 + cc
                            OP(act, lambda c=c, cc=cc, pi=pi, k=k: nc.scalar.activation(
                                out=h2T[:, c, k * 128:(k + 1) * 128], in_=PS[pi][:, cc * 128:(cc + 1) * 128], func=AF.Identity,
                                bias=modc[:, 48 + c:48 + c + 1], scale=sc1[:, 16 + c:16 + c + 1]),
                               reads=[PSB[pi], B_modc, B_sc1], writes=[B_h2T])
                            OP(dve, lambda c=c, cc=cc, pi=pi: nc.vector.tensor_scalar(
                                out=h2f[:, c, :], in0=PS[pi][:, cc * 128:(cc + 1) * 128], scalar1=sc1[:, 16 + c:16 + c + 1],
                                scalar2=modc[:, 48 + c:48 + c + 1], op0=ALU.mult, op1=ALU.add),
                               reads=[PSB[pi], B_modc, B_sc1], writes=[B_h2f])

                    def emit_r():
                        ins = None
                        for c in range(NCH):
                            ins = nc.tensor.matmul(PS[6][:, 0:E], lhsT=h2f[:, c, :], rhs=wrt[:, c, :], start=(c == 0), stop=(c == NCH - 1))
                        return ins
                    OP(pe, emit_r, reads=[B_h2f, B_wrt], writes=[PSB[6]])
                    OP(dve, lambda: nc.vector.tensor_tensor(out=lg[:], in0=PS[6][:, 0:E], in1=brt[:], op=ALU.add), reads=[PSB[6], B_brt], writes=[B_lg])
                    OP(dve, lambda: nc.vector.max(out=m8[:], in_=lg[:]), reads=[B_lg], writes=[B_m8])
                    OP(dve, lambda: nc.vector.tensor_scalar(out=msk[:], in0=lg[:], scalar1=m8[:, 3:4], scalar2=None, op0=ALU.is_ge),
                       reads=[B_lg, B_m8], writes=[B_msk])
                    OP(dve, lambda: nc.vector.tensor_scalar(out=sm[:, 0:1], in0=m8[:, 0:1], scalar1=-1.0, scalar2=None, op0=ALU.mult),
                       reads=[B_m8], writes=[B_sm])
                    OP(act, lambda: nc.scalar.activation(out=eg[:], in_=lg[:], func=AF.Exp, bias=sm[:, 0:1], scale=1.0), reads=[B_lg, B_sm], writes=[B_eg])
                    OP(dve, lambda: nc.vector.tensor_tensor(out=eg[:], in0=eg[:], in1=msk[:], op=ALU.mult), reads=[B_eg, B_msk], writes=[B_eg])
                    OP(dve, lambda: nc.vector.reduce_sum(out=sm[:, 1:2], in_=eg[:], axis=AX.X), reads=[B_eg], writes=[B_sm])
                    OP(dve, lambda: nc.vector.reciprocal(out=sm[:, 2:3], in_=sm[:, 1:2]), reads=[B_sm], writes=[B_sm])
                    OP(dve, lambda k=k: nc.vector.tensor_scalar(out=gates[:, k, :], in0=eg[:], scalar1=sm[:, 2:3], scalar2=None, op0=ALU.mult),
                       reads=[B_eg, B_sm], writes=[B_gates])
                    if "gates" in DBG:
                        DMA(sp, DBG["gates"][k * 128:(k + 1) * 128, :], gates[:, k, :], reads=[B_gates])
                    OP(pe, lambda k=k: nc.tensor.transpose(out=PS[7][0:E, 0:128], in_=gates[:, k, :], identity=ident_f[:]),
                       reads=[B_gates, B_ident_f], writes=[PSB[7]])
                    OP(dve, lambda: nc.vector.tensor_copy(out=gT[:], in_=PS[7][0:E, 0:128]), reads=[PSB[7]], writes=[B_gT])
                    for nt in range(4):
                        pi = nt
                        OP(pe, lambda nt=nt, pi=pi: nc.tensor.matmul(PS[pi][:, :], lhsT=gT[0:E_RUN, :], rhs=bdb[:, nt * 512:(nt + 1) * 512], start=True, stop=True),
                           reads=[B_gT, B_bdb], writes=[PSB[pi]])
                        OP(dve, lambda nt=nt, pi=pi: nc.vector.tensor_tensor(out=tmpc[:], in0=PS[pi][:, :], in1=Gm[:, nt * 512:(nt + 1) * 512], op=ALU.mult),
                           reads=[PSB[pi], B_Gm], writes=[B_tmpc])
                        OP(dve, lambda nt=nt, x1=x1: nc.vector.scalar_tensor_tensor(out=x1[:, nt * 512:(nt + 1) * 512], in0=x1[:, nt * 512:(nt + 1) * 512],
                                                                                     scalar=ALPHA, in1=tmpc[:], op0=ALU.mult, op1=ALU.add),
                           reads=[B_x1, B_tmpc], writes=[B_x1])

            barrier()
            if STOP == "C2":
                raise _Stop()
            with ExitStack() as st2:
                actT = sb(st2, "actT", [128, NCH, 1024], BF16); B_actT = Buf()
                RING = 4
                wring = [sb(st2, f"wring{i}", [128, NCH, 256], BF16) for i in range(RING)]; B_wring = [Buf() for _ in range(RING)]
                gq = [sb(st2, f"gq{i}", [128, 512]) for i in range(2)]; B_gq = [Buf() for _ in range(2)]
                sg = [sb(st2, f"sg{i}", [128, 512]) for i in range(2)]; B_sg = [Buf() for _ in range(2)]
                uq = [sb(st2, f"uq{i}", [128, 512]) for i in range(2)]; B_uq = [Buf() for _ in range(2)]
                t2 = [sb(st2, f"t2{i}", [128, 256]) for i in range(2)]; B_t2 = [Buf() for _ in range(2)]

                wblocks = []
                for e in range(E_RUN):
                    gu_v = w_gu[e].rearrange("(k p) n -> p k n", p=128)
                    dn_v = w_dn[e].rearrange("(k p) n -> p k n", p=128)
                    for sbk in range(8):
                        wblocks.append(gu_v[:, :, sbk * 256:(sbk + 1) * 256])
                        wblocks.append(gu_v[:, :, D + sbk * 256:D + (sbk + 1) * 256])
                    for nb in range(8):
                        wblocks.append(dn_v[:, :, nb * 256:(nb + 1) * 256])
                nload = [0]
                ncons = [0]

                def prefetch():
                    while nload[0] < len(wblocks) and nload[0] < ncons[0] + RING:
                        i = nload[0]
                        DMA(pool, wring[i % RING][:], wblocks[i], writes=[B_wring[i % RING]])
                        nload[0] += 1

                prefetch()
                ev = [0]
                for e in range(E_RUN):
                    for sbk in range(8):
                        ig = ncons[0]
                        iu = ncons[0] + 1
                        sg_, su_ = ig % RING, iu % RING
                        for fc in range(2):
                            fb = sbk * 2 + fc
                            for th in range(2):
                                pp = ev[0] % 2
                                ev[0] += 1
                                pg, pu = 2 * pp, 2 * pp + 1

                                def emit(fc=fc, th=th, pg=pg, pu=pu, sg_=sg_, su_=su_):
                                    ins = None
                                    for c in range(NCH):
                                        nc.tensor.matmul(PS[pg][:, :], lhsT=wring[sg_][:, c, fc * 128:(fc + 1) * 128],
                                                         rhs=h2T[:, c, th * 512:(th + 1) * 512], start=(c == 0), stop=(c == NCH - 1))
                                    for c in range(NCH):
                                        ins = nc.tensor.matmul(PS[pu][:, :], lhsT=wring[su_][:, c, fc * 128:(fc + 1) * 128],
                                                               rhs=h2T[:, c, th * 512:(th + 1) * 512], start=(c == 0), stop=(c == NCH - 1))
                                    return ins
                                OP(pe, emit, reads=[B_wring[sg_], B_wring[su_], B_h2T], writes=[PSB[pg], PSB[pu]])
                                OP(dve, lambda pp=pp, pg=pg, fb=fb, e=e: nc.vector.tensor_scalar(
                                    out=gq[pp][:], in0=PS[pg][:, :], scalar1=bguc[:, e, fb:fb + 1], scalar2=7.0, op0=ALU.add, op1=ALU.min),
                                   reads=[PSB[pg], B_bguc], writes=[B_gq[pp]])
                                OP(act, lambda pp=pp: nc.scalar.activation(out=sg[pp][:], in_=gq[pp][:], func=AF.Sigmoid, scale=1.702),
                                   reads=[B_gq[pp]], writes=[B_sg[pp]])
                                OP(dve, lambda pp=pp, pu=pu, fb=fb, e=e: nc.vector.tensor_scalar(
                                    out=uq[pp][:], in0=PS[pu][:, :], scalar1=bguc[:, e, 16 + fb:16 + fb + 1], scalar2=7.0, op0=ALU.add, op1=ALU.min),
                                   reads=[PSB[pu], B_bguc], writes=[B_uq[pp]])
                                OP(dve, lambda pp=pp: nc.vector.tensor_scalar(out=uq[pp][:], in0=uq[pp][:], scalar1=-7.0, scalar2=1.0, op0=ALU.max, op1=ALU.add),
                                   reads=[B_uq[pp]], writes=[B_uq[pp]])
                                OP(dve, lambda pp=pp: nc.vector.tensor_tensor(out=gq[pp][:], in0=gq[pp][:], in1=sg[pp][:], op=ALU.mult),
                                   reads=[B_gq[pp], B_sg[pp]], writes=[B_gq[pp]])
                                OP(dve, lambda pp=pp, fb=fb, th=th: nc.vector.tensor_tensor(out=actT[:, fb, th * 512:(th + 1) * 512], in0=gq[pp][:], in1=uq[pp][:],
                                                                                              op=ALU.mult),
                                   reads=[B_gq[pp], B_uq[pp]], writes=[B_actT])
                        ncons[0] += 2
                        prefetch()
                    for nb in range(8):
                        idn = ncons[0]
                        sd = idn % RING
                        for tl in range(NT_OWN):
                            pi = 4 + (ev[0] % 4)
                            tq = ev[0] % 2
                            ev[0] += 1

                            def emit(tl=tl, pi=pi, sd=sd):
                                ins = None
                                for c in range(NCH):
                                    ins = nc.tensor.matmul(PS[pi][:, 0:256], lhsT=actT[:, c, tl * 128:(tl + 1) * 128], rhs=wring[sd][:, c, :],
                                                           start=(c == 0), stop=(c == NCH - 1))
                                return ins
                            OP(pe, emit, reads=[B_actT, B_wring[sd]], writes=[PSB[pi]])
                            OP(dve, lambda tl=tl, pi=pi, tq=tq, nb=nb, e=e: nc.vector.scalar_tensor_tensor(
                                out=t2[tq][:], in0=PS[pi][:, 0:256], scalar=gates[:, tl, e:e + 1], in1=Gm[:, nb * 256:(nb + 1) * 256],
                                op0=ALU.mult, op1=ALU.mult),
                               reads=[PSB[pi], B_gates, B_Gm], writes=[B_t2[tq]])
                            OP(dve, lambda tl=tl, tq=tq, nb=nb: nc.vector.tensor_tensor(
                                out=acc[:, tl, nb * 256:(nb + 1) * 256], in0=acc[:, tl, nb * 256:(nb + 1) * 256], in1=t2[tq][:], op=ALU.add),
                               reads=[B_t2[tq], B_acc[tl]], writes=[B_acc[tl]])
                        ncons[0] += 1
                        prefetch()

            barrier()
            if STOP == "D":
                raise _Stop()
            with ExitStack() as st2:
                l2g = sb(st2, "l2g", [128, D]); B_l2g = Buf()
                l2b = sb(st2, "l2b", [128, D]); B_l2b = Buf()
                ot = [sb(st2, f"ot{i}", [128, D]) for i in range(2)]; B_ot = [Buf() for _ in range(2)]
                stats = sb(st2, "stats2", [128, 4, 6]); B_stats = Buf()
                mv = sb(st2, "mv2", [128, 4]); B_mv = Buf()
                DMA(sp, l2g[:], ln2_g[0].partition_broadcast(128), writes=[B_l2g])
                DMA(sp, l2b[:], ln2_b[0].partition_broadcast(128), writes=[B_l2b])
                last = []
                for k in range(NT_OWN):
                    o2 = k % 2
                    src = acc[:, k, :]
                    dst = ot[o2][:]
                    for q in range(4):
                        OP(dve, lambda q=q, src=src: nc.vector.bn_stats(out=stats[:, q, :], in_=src[:, q * 512:(q + 1) * 512]), reads=[B_acc[k]], writes=[B_stats])
                    OP(dve, lambda: nc.vector.bn_aggr(out=mv[:, 0:2], in_=stats[:, :, :].rearrange("p a b -> p (a b)")), reads=[B_stats], writes=[B_mv])
                    OP(act, lambda: nc.scalar.activation(out=mv[:, 3:4], in_=mv[:, 1:2], func=AF.Sqrt, bias=eps_c[:, 0:1], scale=1.0),
                       reads=[B_mv, B_epsc], writes=[B_mv])
                    OP(dve, lambda: nc.vector.reciprocal(out=mv[:, 2:3], in_=mv[:, 3:4]), reads=[B_mv], writes=[B_mv])
                    OP(dve, lambda src=src, dst=dst: nc.vector.tensor_scalar(out=dst, in0=src, scalar1=mv[:, 0:1], scalar2=mv[:, 2:3], op0=ALU.subtract, op1=ALU.mult),
                       reads=[B_acc[k], B_mv], writes=[B_ot[o2]])
                    OP(dve, lambda dst=dst: nc.vector.tensor_tensor(out=dst, in0=dst, in1=l2g[:], op=ALU.mult), reads=[B_ot[o2], B_l2g], writes=[B_ot[o2]])
                    OP(dve, lambda dst=dst: nc.vector.tensor_tensor(out=dst, in0=dst, in1=l2b[:], op=ALU.add), reads=[B_ot[o2], B_l2b], writes=[B_ot[o2]])
                    last.append(DMA(sp, y[k * 128:(k + 1) * 128, :], ot[o2][:], reads=[B_ot[o2]]))
                sp.wait(*last)
    except _Stop:
        pass
    for i, sem in enumerate(sp.ring):
        if sp.rcnt[i]:
            sp.wait((sem, 16 * sp.rcnt[i], f"sp_r{i}"))
    return nc


def _consts():
    ql = np.arange(128)[:, None]
    col = np.arange(17 * 128)[None, :]
    i = col // 128
    kl = col % 128
    delta = (16 - i) * 128 + ql - kl
    mult = ((delta >= 0) & (delta <= 128)).astype(np.float32) \
        + ((delta >= 0) & (delta % 4 == 0) & (delta <= 512)).astype(np.float32) \
        + ((delta >= 0) & (delta % 16 == 0) & (delta <= 2048)).astype(np.float32)
    cm4 = np.zeros((128, 512), np.float32)
    kk = np.arange(128)[None, :]
    cm4[:, 384:512] = np.where(kk > ql, NEG, 0.0)
    return mult.astype(np.float32), delta.astype(np.float32), cm4


def make_in_maps(inputs):
    f = lambda a: np.ascontiguousarray(np.asarray(a, dtype=np.float32))
    x = f(inputs["x"])
    c = f(inputs["c"])
    mult, delta, cm4 = _consts()
    shared = {
        "w_ada": f(inputs["w_ada"][0]),
        "b_ada_c": f(np.asarray(inputs["b_ada"][0]).reshape(96, 128).T),
        "w_in": f(inputs["w_in"][0]),
        "bf_c": f(np.asarray(inputs["b_forget"][0]).reshape(8, 1)),
        "w_out": f(inputs["w_out"][0]),
        "ln1_g": f(inputs["ln1_g"]), "ln1_b": f(inputs["ln1_b"]),
        "w_router": f(inputs["w_router"][0]),
        "b_router": f(inputs["b_router"]),
        "w_gu": f(inputs["w_gate_up"][0][:E_RUN]),
        "b_gu_c": f(np.asarray(inputs["b_gate_up"][0]).reshape(E, 32, 128).transpose(2, 0, 1)),
        "w_dn": f(inputs["w_down"][0][:E_RUN]),
        "b_dn": f(inputs["b_down"][0][:E_RUN]),
        "ln2_g": f(inputs["ln2_g"]), "ln2_b": f(inputs["ln2_b"]),
        "ident": np.eye(128, dtype=np.float32),
        "dmult": mult, "ddelta": delta, "cm4": cm4,
    }
    maps = []
    for i in range(8):
        b, j = i // 4, i % 4
        xs = np.zeros((S_LOC, D), np.float32)
        g0 = 128 * (j - 3)
        lo = max(0, -g0)
        xs[lo:S_LOC] = x[b, g0 + lo:g0 + S_LOC]
        kv = np.zeros((1, S_LOC), np.float32)
        kv[0, :lo] = NEG
        m = dict(shared)
        m["xs"] = xs
        m["kval"] = kv
        m["cb"] = f(c[b].reshape(NCH, 128).T)
        maps.append(m)
    return maps


def kernel(**inputs):
    nc = build_nc()
    maps = make_in_maps(inputs)
    res = run_bass_kernel_spmd(nc, maps, core_ids=list(range(8)))
    out = np.zeros((2, 4096, D), np.float32)
    for i in range(8):
        b, j = i // 4, i % 4
        yi = np.asarray(res.results[i]["y"]).reshape(NT_OWN, 128, D)
        for k in range(NT_OWN):
            g = (4 * k + j) * 128
            out[b, g:g + 128] = yi[k]
    return out
```

```python
import math
import numpy as np
import concourse.bass as bass
import concourse.mybir as mybir
from concourse.bass_utils import run_bass_kernel_spmd

F32 = mybir.dt.float32
BF16 = mybir.dt.bfloat16
AF = mybir.ActivationFunctionType
ALU = mybir.AluOpType
AX = mybir.AxisListType

D = 2048
NCH = 16
S_LOC = 4096
NT_OWN = 8
E = 32
E_RUN = 32
NEG = -30000.0
SQ = math.sqrt(128.0)
ISQ = 1.0 / SQ
ALPHA = 2.0 ** 0.25
EPS = 1e-5
DBG = {}
STOP = None


class _Stop(Exception):
    pass


class Eng:
    def __init__(self, nc, h, name, nring=0):
        self.nc = nc
        self.h = h
        self.name = name
        self.sem = nc.semaphore(name + "_s").__enter__()
        self.n = 0
        self.seen = {}
        self.ring = [nc.semaphore(f"{name}_r{i}").__enter__() for i in range(nring)]
        self.rcnt = [0] * nring
        self.rnext = 0

    def wait(self, *toks):
        for t in toks:
            if t is None:
                continue
            sem, v, key = t
            if self.seen.get(key, 0) >= v:
                continue
            self.h.wait_ge(sem, v)
            self.seen[key] = v

    def sig(self, ins):
        self.n += 1
        ins.then_inc(self.sem, 1)
        return (self.sem, self.n, self.name)

    def dma_sig(self, ins):
        i = self.rnext
        self.rnext = (self.rnext + 1) % len(self.ring)
        self.rcnt[i] += 1
        ins.then_inc(self.ring[i], 16)
        return (self.ring[i], 16 * self.rcnt[i], f"{self.name}_r{i}")


class Buf:
    def __init__(self):
        self.w = None
        self.r = {}


def _deps(eng, reads, writes):
    for b in reads:
        eng.wait(b.w)
    for b in writes:
        eng.wait(b.w, *b.r.values())


def _commit(tok, reads, writes):
    for b in reads:
        old = b.r.get(tok[2])
        if old is None or old[1] < tok[1]:
            b.r[tok[2]] = tok
    for b in writes:
        b.w = tok
        b.r = {}


def OP(eng, emit, reads=(), writes=()):
    _deps(eng, reads, writes)
    ins = emit()
    tok = eng.sig(ins)
    _commit(tok, reads, writes)
    return tok


def DMA(eng, out, in_, reads=(), writes=()):
    _deps(eng, reads, writes)
    i = eng.rnext
    if eng.rcnt[i]:
        eng.wait((eng.ring[i], 16 * eng.rcnt[i], f"{eng.name}_r{i}"))
    ins = eng.h.dma_start(out=out, in_=in_)
    tok = eng.dma_sig(ins)
    _commit(tok, reads, writes)
    return tok


def build_nc():
    nc = bass.Bass("TRN2", target_bir_lowering=False)

    def din(name, shape, dt=F32):
        return nc.dram_tensor(name, list(shape), dt, kind="ExternalInput").ap()

    xs = din("xs", [S_LOC, D])
    kval = din("kval", [1, S_LOC])
    cb = din("cb", [128, NCH])
    w_ada = din("w_ada", [D, 6 * D])
    b_ada_c = din("b_ada_c", [128, 96])
    w_in = din("w_in", [D, 6152])
    bf_c = din("bf_c", [8, 1])
    w_out = din("w_out", [D, D])
    ln1_g = din("ln1_g", [1, D])
    ln1_b = din("ln1_b", [1, D])
    w_router = din("w_router", [D, E])
    b_router = din("b_router", [1, E])
    w_gu = din("w_gu", [E_RUN, D, 2 * D])
    b_gu_c = din("b_gu_c", [128, E, 32])
    w_dn = din("w_dn", [E_RUN, D, D])
    b_dn = din("b_dn", [E_RUN, D])
    ln2_g = din("ln2_g", [1, D])
    ln2_b = din("ln2_b", [1, D])
    ident_d = din("ident", [128, 128])
    dmult_d = din("dmult", [128, 17 * 128])
    ddelta_d = din("ddelta", [128, 17 * 128])
    cm4_d = din("cm4", [128, 512])
    y = nc.dram_tensor("y", [NT_OWN * 128, D], F32, kind="ExternalOutput").ap()

    KT_scr = nc.dram_tensor("KT_scr", [16, 128, S_LOC], BF16, kind="Internal").ap()
    QT_scr = nc.dram_tensor("QT_scr", [16, 128, 1024], BF16, kind="Internal").ap()
    V_scr = nc.dram_tensor("V_scr", [8, 128, 32, 256], BF16, kind="Internal").ap()

    pe = Eng(nc, nc.tensor, "pe")
    act = Eng(nc, nc.scalar, "act")
    dve = Eng(nc, nc.vector, "dve")
    pool = Eng(nc, nc.gpsimd, "pool", nring=8)
    sp = Eng(nc, nc.sync, "sp", nring=8)

    from contextlib import ExitStack
    glob = ExitStack()

    def barrier():
        toks = [(e.sem, e.n, e.name) for e in (pe, act, dve) if e.n > 0]
        for q in (sp, pool):
            for i, sem in enumerate(q.ring):
                if q.rcnt[i]:
                    toks.append((sem, 16 * q.rcnt[i], f"{q.name}_r{i}"))
        for e in (pe, act, dve, pool, sp):
            e.wait(*toks)

    def sb(stack, name, shape, dt=F32):
        return stack.enter_context(nc.sbuf_tensor(name, list(shape), dt))

    PS = [glob.enter_context(nc.psum_tensor(f"ps{i}", [128, 512], F32)) for i in range(8)]
    PSB = [Buf() for _ in range(8)]

    ident_f = sb(glob, "ident_f", [128, 128]); B_ident_f = Buf()
    ident_b = sb(glob, "ident_b", [128, 128], BF16); B_ident_b = Buf()
    ones_f = sb(glob, "ones_f", [128, 128]); B_ones_f = Buf()
    ones_b = sb(glob, "ones_b", [128, 128], BF16); B_ones_b = Buf()
    modc = sb(glob, "modc", [128, 96]); B_modc = Buf()
    sc1 = sb(glob, "sc1", [128, 32]); B_sc1 = Buf()
    cbt = sb(glob, "cbt", [128, NCH]); B_cbt = Buf()
    cond_b = sb(glob, "cond_b", [128, NCH], BF16); B_cond = Buf()
    badac = sb(glob, "badac", [128, 96]); B_badac = Buf()
    eps_c = sb(glob, "eps_c", [128, 1]); B_epsc = Buf()
    ab = ExitStack()
    nchi = sb(ab, "nchi", [8, S_LOC], BF16)
    ncmid = sb(ab, "ncmid", [8, S_LOC], BF16)
    nclo = sb(ab, "nclo", [8, S_LOC], BF16)
    B_ncs = Buf()
    sel = sb(ab, "sel", [8, 8, 128], BF16); B_sel = Buf()
    kval_b = sb(ab, "kval_b", [1, S_LOC], BF16); B_kvalb = Buf()
    MT_scr = nc.dram_tensor("MT_scr", [128, NCH, 1024], BF16, kind="Internal").ap()

    DMA(sp, ident_f[:], ident_d[:, :], writes=[B_ident_f])
    DMA(sp, cbt[:], cb[:, :], writes=[B_cbt])
    DMA(sp, badac[:], b_ada_c[:, :], writes=[B_badac])
    OP(dve, lambda: nc.vector.tensor_copy(out=ident_b[:], in_=ident_f[:]), reads=[B_ident_f], writes=[B_ident_b])
    OP(dve, lambda: nc.vector.memset(eps_c[:], EPS), writes=[B_epsc])
    OP(dve, lambda: nc.vector.memset(ones_f[:], 1.0), writes=[B_ones_f])
    OP(dve, lambda: nc.vector.memset(ones_b[:], 1.0), writes=[B_ones_b])
    for h in range(8):
        OP(dve, lambda h=h: nc.vector.tensor_copy(out=sel[:, h, :], in_=ident_f[0:8, h:h + 1].to_broadcast([8, 128])),
           reads=[B_ident_f], writes=[B_sel])
    OP(act, lambda: nc.scalar.activation(out=cond_b[:], in_=cbt[:], func=AF.Silu), reads=[B_cbt], writes=[B_cond])

    try:
        with ExitStack() as st:
            wa = [sb(st, f"wa{i}", [128, NCH, 512], BF16) for i in range(2)]
            B_wa = [Buf() for _ in range(2)]
            w_ada_v = w_ada.rearrange("(k p) n -> p k n", p=128)
            NB = 24
            DMA(pool, wa[0][:], w_ada_v[:, :, 0:512], writes=[B_wa[0]])
            for blk in range(NB):
                if blk + 1 < NB:
                    s = (blk + 1) % 2
                    DMA(pool, wa[s][:], w_ada_v[:, :, (blk + 1) * 512:(blk + 2) * 512], writes=[B_wa[s]])
                s = blk % 2

                def emit(blk=blk, s=s):
                    ins = None
                    for nchk in range(4):
                        col = blk * 4 + nchk
                        for k in range(NCH):
                            ins = nc.tensor.matmul(PS[0][:, col:col + 1], lhsT=wa[s][:, k, nchk * 128:(nchk + 1) * 128],
                                                   rhs=cond_b[:, k:k + 1], start=(k == 0), stop=(k == NCH - 1))
                    return ins
                OP(pe, emit, reads=[B_wa[s], B_cond], writes=[PSB[0]])
            OP(dve, lambda: nc.vector.tensor_tensor(out=modc[:], in0=PS[0][:, 0:96], in1=badac[:], op=ALU.add),
               reads=[PSB[0], B_badac], writes=[B_modc])
            OP(dve, lambda: nc.vector.tensor_scalar(out=sc1[:, 0:16], in0=modc[:, 16:32], scalar1=1.0, scalar2=None, op0=ALU.add),
               reads=[B_modc], writes=[B_sc1])
            OP(dve, lambda: nc.vector.tensor_scalar(out=sc1[:, 16:32], in0=modc[:, 64:80], scalar1=1.0, scalar2=None, op0=ALU.add),
               reads=[B_modc], writes=[B_sc1])
        if "modc" in DBG:
            DMA(sp, DBG["modc"][:, :], modc[:], reads=[B_modc])
        barrier()
        if STOP == "p0":
            raise _Stop()

        w_in_v = w_in.rearrange("(k p) n -> p k n", p=128)
        with ExitStack() as st:
            hT = sb(st, "hT", [128, NCH, 2048], BF16); B_hT = Buf()
            xt = [sb(st, f"xt{i}", [128, D]) for i in range(2)]; B_xt = [Buf() for _ in range(2)]
            wr_ = [sb(st, f"wi{i}", [128, NCH, 256], BF16) for i in range(3)]; B_wr = [Buf() for _ in range(3)]
            kst = [sb(st, f"kst{i}", [128, 2048], BF16) for i in range(2)]; B_kst = [Buf() for _ in range(2)]
            qst = [sb(st, f"qst{i}", [128, 512], BF16) for i in range(2)]; B_qst = [Buf() for _ in range(2)]
            vst = [sb(st, f"vst{i}", [128, 16, 256], BF16) for i in range(2)]; B_vst = [Buf() for _ in range(2)]
            wf = sb(st, "wf", [128, NCH, 8], BF16); B_wf = Buf()
            bfc = sb(st, "bfc", [8, 1]); B_bfc = Buf()
            ftmp = [sb(st, f"ftmp{i}", [8, 512]) for i in range(4)]; B_ft = [Buf() for _ in range(4)]
            kval8 = sb(st, "kval8", [8, S_LOC]); B_kval8 = Buf()
            DMA(sp, kval8[:], kval[0].partition_broadcast(8), writes=[B_kval8])
            OP(dve, lambda: nc.vector.tensor_copy(out=kval_b[:], in_=kval8[0:1, :]), reads=[B_kval8], writes=[B_kvalb])
            rawc = [sb(st, f"rawc{i}", [8, 512]) for i in range(2)]; B_rawc = [Buf() for _ in range(2)]
            ones8 = sb(st, "ones8", [8, 512]); B_ones8 = Buf()
            one_c = sb(st, "one_c", [8, 1]); B_onec = Buf()
            OP(dve, lambda: nc.vector.memset(ones8[:], 1.0), writes=[B_ones8])
            OP(dve, lambda: nc.vector.memset(one_c[:], 1.0), writes=[B_onec])
            DMA(sp, bfc[:], bf_c[:, :], writes=[B_bfc])
            with nc.allow_non_contiguous_dma(reason="tiny forget-gate weight slice"):
                DMA(pool, wf[:], w_in_v[:, :, 6144:6152], writes=[B_wf])

            blocks = []
            for g, (qo, ko, vo, hbase) in enumerate([(0, 1024, 2048, 0), (3072, 4096, 5120, 8)]):
                for hp in range(4):
                    blocks.append(("q", hbase + 2 * hp, qo + hp * 256))
                    blocks.append(("k", hbase + 2 * hp, ko + hp * 256))
                    blocks.append(("v", g * 4 + hp, vo + hp * 256))
            psrot = [0]

            def next_ps(lo=0, n=4):
                i = lo + psrot[0] % n
                psrot[0] += 1
                return i

            cnt = {"k": 0, "q": 0, "v": 0, "w": 0, "scan": 0}
            for half in range(2):
                for tt in range(16):
                    s = tt % 2
                    DMA(sp, xt[s][:], xs[(half * 16 + tt) * 128:(half * 16 + tt + 1) * 128, :], writes=[B_xt[s]])
                    for q4 in range(4):
                        pi = next_ps(0, 4)

                        def emit(q4=q4, s=s, pi=pi):
                            ins = None
                            for cc in range(4):
                                c = q4 * 4 + cc
                                ins = nc.tensor.transpose(out=PS[pi][:, cc * 128:(cc + 1) * 128], in_=xt[s][:, c * 128:(c + 1) * 128],
                                                          identity=ident_f[:])
                            return ins
                        OP(pe, emit, reads=[B_xt[s], B_ident_f], writes=[PSB[pi]])
                        for cc in range(4):
                            c = q4 * 4 + cc
                            if cc % 2 == 0:
                                OP(act, lambda c=c, cc=cc, pi=pi, tt=tt: nc.scalar.activation(
                                    out=hT[:, c, tt * 128:(tt + 1) * 128], in_=PS[pi][:, cc * 128:(cc + 1) * 128],
                                    func=AF.Identity, bias=modc[:, c:c + 1], scale=sc1[:, c:c + 1]),
                                   reads=[PSB[pi], B_modc, B_sc1], writes=[B_hT])
                            else:
                                OP(dve, lambda c=c, cc=cc, pi=pi, tt=tt: nc.vector.tensor_scalar(
                                    out=hT[:, c, tt * 128:(tt + 1) * 128], in0=PS[pi][:, cc * 128:(cc + 1) * 128],
                                    scalar1=sc1[:, c:c + 1], scalar2=modc[:, c:c + 1], op0=ALU.mult, op1=ALU.add),
                                   reads=[PSB[pi], B_modc, B_sc1], writes=[B_hT])
                nblk = len(blocks)

                def load_w(bi):
                    s = cnt["w"] % 3
                    cnt["w"] += 1
                    col = blocks[bi][2]
                    DMA(pool, wr_[s][:], w_in_v[:, :, col:col + 256], writes=[B_wr[s]])
                    return s
                slots = {}
                slots[0] = load_w(0)
                slots[1] = load_w(1)
                for bi in range(nblk):
                    if bi + 2 < nblk:
                        slots[bi + 2] = load_w(bi + 2)
                    kind, hidx, col = blocks[bi]
                    ws = slots[bi]
                    if kind == "k":
                        for hh in range(2):
                            ks = cnt["k"] % 2
                            cnt["k"] += 1
                            for tc in range(4):
                                pi = next_ps(0, 4)

                                def emit(hh=hh, tc=tc, pi=pi, ws=ws):
                                    ins = None
                                    for c in range(NCH):
                                        ins = nc.tensor.matmul(PS[pi][:, :], lhsT=wr_[ws][:, c, hh * 128:(hh + 1) * 128],
                                                               rhs=hT[:, c, tc * 512:(tc + 1) * 512], start=(c == 0), stop=(c == NCH - 1))
                                    return ins
                                OP(pe, emit, reads=[B_wr[ws], B_hT], writes=[PSB[pi]])
                                if tc % 2 == 0:
                                    OP(act, lambda ks=ks, tc=tc, pi=pi: nc.scalar.copy(out=kst[ks][:, tc * 512:(tc + 1) * 512], in_=PS[pi][:, :]),
                                       reads=[PSB[pi]], writes=[B_kst[ks]])
                                else:
                                    OP(dve, lambda ks=ks, tc=tc, pi=pi: nc.vector.tensor_copy(out=kst[ks][:, tc * 512:(tc + 1) * 512], in_=PS[pi][:, :]),
                                       reads=[PSB[pi]], writes=[B_kst[ks]])
                            DMA(sp, KT_scr[hidx + hh, :, half * 2048:(half + 1) * 2048], kst[ks][:], reads=[B_kst[ks]])
                    elif kind == "q":
                        for hh in range(2):
                            qs = cnt["q"] % 2
                            cnt["q"] += 1
                            pi = next_ps(0, 4)
                            hT4 = hT[:, :, :].rearrange("p c (b t) -> p c b t", t=128)

                            def emit(hh=hh, pi=pi, ws=ws):
                                ins = None
                                for kk in range(4):
                                    for c in range(NCH):
                                        ins = nc.tensor.matmul(PS[pi][:, kk * 128:(kk + 1) * 128], lhsT=wr_[ws][:, c, hh * 128:(hh + 1) * 128],
                                                               rhs=hT[:, c, (4 * kk + 3) * 128:(4 * kk + 4) * 128], start=(c == 0), stop=(c == NCH - 1))
                                return ins
                            OP(pe, emit, reads=[B_wr[ws], B_hT], writes=[PSB[pi]])
                            OP(act, lambda qs=qs, pi=pi: nc.scalar.copy(out=qst[qs][:, :], in_=PS[pi][:, :]), reads=[PSB[pi]], writes=[B_qst[qs]])
                            DMA(sp, QT_scr[hidx + hh, :, half * 512:(half + 1) * 512], qst[qs][:], reads=[B_qst[qs]])
                    else:
                        vs = cnt["v"] % 2
                        cnt["v"] += 1
                        for tp in range(8):
                            pi = next_ps(0, 4)

                            def emit(tp=tp, pi=pi, ws=ws):
                                ins = None
                                for t2 in range(2):
                                    tt = tp * 2 + t2
                                    for c in range(NCH):
                                        ins = nc.tensor.matmul(PS[pi][:, t2 * 256:(t2 + 1) * 256], lhsT=hT[:, c, tt * 128:(tt + 1) * 128],
                                                               rhs=wr_[ws][:, c, :], start=(c == 0), stop=(c == NCH - 1))
                                return ins
                            OP(pe, emit, reads=[B_wr[ws], B_hT], writes=[PSB[pi]])
                            if tp % 2 == 0:
                                OP(act, lambda vs=vs, tp=tp, pi=pi: nc.scalar.copy(
                                    out=vst[vs][:, 2 * tp:2 * tp + 2, :], in_=PS[pi][:, :].rearrange("p (a b) -> p a b", a=2)),
                                   reads=[PSB[pi]], writes=[B_vst[vs]])
                            else:
                                OP(dve, lambda vs=vs, tp=tp, pi=pi: nc.vector.tensor_copy(
                                    out=vst[vs][:, 2 * tp:2 * tp + 2, :], in_=PS[pi][:, :].rearrange("p (a b) -> p a b", a=2)),
                                   reads=[PSB[pi]], writes=[B_vst[vs]])
                        DMA(sp, V_scr[hidx, :, half * 16:(half + 1) * 16, :], vst[vs][:], reads=[B_vst[vs]])
                for tc in range(4):
                    pi = 4 + (tc % 2)
                    gc0 = half * 2048 + tc * 512

                    def emit(tc=tc, pi=pi):
                        ins = None
                        for c in range(NCH):
                            ins = nc.tensor.matmul(PS[pi][0:8, :], lhsT=wf[:, c, 0:8], rhs=hT[:, c, tc * 512:(tc + 1) * 512],
                                                   start=(c == 0), stop=(c == NCH - 1))
                        return ins
                    OP(pe, emit, reads=[B_wf, B_hT], writes=[PSB[pi]])
                    yv, av, lv, rv = ftmp
                    By, Ba, Bl, Br = B_ft
                    ev, Be, t5, B5 = av, Ba, rv, Br
                    OP(dve, lambda pi=pi: nc.vector.tensor_scalar(out=yv[:], in0=PS[pi][0:8, :], scalar1=bfc[:, 0:1], scalar2=None, op0=ALU.add),
                       reads=[PSB[pi], B_bfc], writes=[By])
                    OP(act, lambda: nc.scalar.activation(out=av[:], in_=yv[:], func=AF.Abs), reads=[By], writes=[Ba])
                    OP(act, lambda: nc.scalar.activation(out=ev[:], in_=av[:], func=AF.Exp, scale=-1.0), reads=[Ba], writes=[Be])
                    OP(act, lambda: nc.scalar.activation(out=lv[:], in_=ev[:], func=AF.Ln, bias=one_c[:, 0:1], scale=1.0),
                       reads=[Be, B_onec], writes=[Bl])
                    OP(dve, lambda: nc.vector.tensor_scalar(out=rv[:], in0=yv[:], scalar1=0.0, scalar2=None, op0=ALU.min), reads=[By], writes=[Br])
                    OP(dve, lambda: nc.vector.tensor_tensor(out=lv[:], in0=lv[:], in1=rv[:], op=ALU.subtract), reads=[Bl, Br], writes=[Bl])
                    sidx = cnt["scan"]
                    cnt["scan"] += 1
                    cur, prev = rawc[sidx % 2], rawc[(sidx + 1) % 2]
                    Bcur, Bprev = B_rawc[sidx % 2], B_rawc[(sidx + 1) % 2]
                    if sidx == 0:
                        OP(dve, lambda cur=cur: nc.vector.tensor_tensor_scan(out=cur[:], data0=ones8[:], data1=lv[:], initial=0.0,
                                                                             op0=ALU.mult, op1=ALU.add),
                           reads=[Bl, B_ones8], writes=[Bcur])
                    else:
                        OP(dve, lambda cur=cur, prev=prev: nc.vector.tensor_tensor_scan(out=cur[:], data0=ones8[:], data1=lv[:],
                                                                                        initial=prev[:, 511:512], op0=ALU.mult, op1=ALU.add),
                           reads=[Bl, B_ones8, Bprev], writes=[Bcur])
                    OP(dve, lambda cur=cur, gc0=gc0: nc.vector.scalar_tensor_tensor(out=t5[:], in0=cur[:], scalar=SQ, in1=kval8[:, gc0:gc0 + 512],
                                                                                     op0=ALU.mult, op1=ALU.add),
                       reads=[Bcur, B_kval8], writes=[B5])
                    OP(dve, lambda gc0=gc0: nc.vector.tensor_copy(out=nchi[:, gc0:gc0 + 512], in_=t5[:]), reads=[B5], writes=[B_ncs])
                    OP(dve, lambda gc0=gc0: nc.vector.tensor_tensor(out=t5[:], in0=t5[:], in1=nchi[:, gc0:gc0 + 512], op=ALU.subtract),
                       reads=[B5, B_ncs], writes=[B5])
                    OP(dve, lambda gc0=gc0: nc.vector.tensor_copy(out=ncmid[:, gc0:gc0 + 512], in_=t5[:]), reads=[B5], writes=[B_ncs])
                    OP(dve, lambda gc0=gc0: nc.vector.tensor_tensor(out=t5[:], in0=t5[:], in1=ncmid[:, gc0:gc0 + 512], op=ALU.subtract),
                       reads=[B5, B_ncs], writes=[B5])
                    OP(dve, lambda gc0=gc0: nc.vector.tensor_copy(out=nclo[:, gc0:gc0 + 512], in_=t5[:]), reads=[B5], writes=[B_ncs])

        barrier()
        if STOP == "A":
            raise _Stop()
        with ExitStack() as st:
            KT = [sb(st, f"KT{i}", [128, S_LOC], BF16) for i in range(2)]; B_KT = [Buf() for _ in range(2)]
            Vh = [sb(st, f"Vh{i}", [128, 32, 128], BF16) for i in range(2)]; B_Vh = [Buf() for _ in range(2)]
            QT = [sb(st, f"QT{i}", [128, 1024], BF16) for i in range(2)]; B_QT = [Buf() for _ in range(2)]
            T0 = sb(st, "T0", [128, 2176]); B_T0 = Buf()
            DS = sb(st, "DS", [128, 2176]); B_DS = Buf()
            Bt = sb(st, "Bt", [128, 2176]); B_Bt = Buf()
            Bhi = sb(st, "Bhi", [128, 2176], BF16); B_Bhi = Buf()
            Blo = sb(st, "Blo", [128, 2176], BF16); B_Blo = Buf()
            cm4 = sb(st, "cm4_sb", [128, 512], BF16); B_cm4 = Buf()
            cm4f = sb(st, "cm4f", [128, 512]); B_cm4f = Buf()
            Pm = [sb(st, f"Pm{i}", [128, S_LOC], BF16) for i in range(2)]; B_Pm = [Buf() for _ in range(2)]
            PT = [sb(st, f"PT{i}", [128, 4, 128], BF16) for i in range(4)]; B_PT = [Buf() for _ in range(4)]
            mx = [sb(st, f"mx{i}", [128, 16]) for i in range(2)]; B_mx = [Buf() for _ in range(2)]
            rs = [sb(st, f"rs{i}", [128, 16]) for i in range(2)]; B_rs = [Buf() for _ in range(2)]
            osb = [sb(st, f"osb{i}", [128, 128], BF16) for i in range(2)]; B_osb = [Buf() for _ in range(2)]
            mixT = sb(st, "mixT", [128, NCH, 1024], BF16); B_mixT = [Buf() for _ in range(NCH)]

            DMA(sp, T0[:], dmult_d[:, :], writes=[B_T0])
            DMA(sp, DS[:], ddelta_d[:, :], writes=[B_DS])
            DMA(sp, cm4f[:], cm4_d[:, :], writes=[B_cm4f])
            OP(dve, lambda: nc.vector.tensor_copy(out=cm4[:], in_=cm4f[:]), reads=[B_cm4f], writes=[B_cm4])
            OP(dve, lambda: nc.vector.tensor_scalar(out=Bt[:], in0=T0[:], scalar1=0.5, scalar2=NEG, op0=ALU.is_lt, op1=ALU.mult),
               reads=[B_T0], writes=[B_Bt])
            OP(dve, lambda: nc.vector.tensor_scalar(out=T0[:], in0=T0[:], scalar1=1.0, scalar2=None, op0=ALU.max), reads=[B_T0], writes=[B_T0])
            OP(act, lambda: nc.scalar.activation(out=T0[:], in_=T0[:], func=AF.Ln), reads=[B_T0], writes=[B_T0])
            OP(dve, lambda: nc.vector.scalar_tensor_tensor(out=T0[:], in0=T0[:], scalar=SQ, in1=Bt[:], op0=ALU.mult, op1=ALU.add),
               reads=[B_T0, B_Bt], writes=[B_T0])
            OP(dve, lambda: nc.vector.tensor_scalar(out=DS[:], in0=DS[:], scalar1=SQ, scalar2=None, op0=ALU.mult), reads=[B_DS], writes=[B_DS])

            def load_head(hd):
                s = hd % 2
                DMA(sp, KT[s][:], KT_scr[hd, :, :], writes=[B_KT[s]])
                DMA(sp, QT[s][:], QT_scr[hd, :, :], writes=[B_QT[s]])
                with nc.allow_non_contiguous_dma(reason="per-head V slice, 256B rows"):
                    DMA(sp, Vh[s][:], V_scr[hd // 2, :, :, (hd % 2) * 128:(hd % 2 + 1) * 128], writes=[B_Vh[s]])

            load_head(0)
            qcnt = [0]
            ptc = [0]
            for hd in range(16):
                if hd + 1 < 16:
                    load_head(hd + 1)
                s = hd % 2
                is_fox = hd >= 8
                if not is_fox:
                    slope = 2.0 ** (-(hd + 1))
                    OP(dve, lambda slope=slope: nc.vector.scalar_tensor_tensor(out=Bt[:], in0=DS[:], scalar=-slope, in1=T0[:], op0=ALU.mult, op1=ALU.add),
                       reads=[B_DS, B_T0], writes=[B_Bt])
                    OP(dve, lambda: nc.vector.tensor_copy(out=Bhi[:], in_=Bt[:]), reads=[B_Bt], writes=[B_Bhi])
                    OP(dve, lambda: nc.vector.tensor_tensor(out=Blo[:], in0=Bt[:], in1=Bhi[:], op=ALU.subtract), reads=[B_Bt, B_Bhi], writes=[B_Blo])
                for k in range(NT_OWN):
                    qi = qcnt[0]
                    qcnt[0] += 1
                    q2 = qi % 2
                    qb = 4 * k + 3
                    if is_fox:
                        chunks = [(4 * c, 4, None) for c in range(k + 1)]
                    else:
                        chunks = [(4 * c, 4, 128 * (13 - 4 * (k - c))) for c in range(max(0, k - 3), k + 1)]
                        if k >= 4:
                            chunks.append((qb - 16, 1, 0))
                    nchk = len(chunks)

                    def s_emit(pi, ci, light=False):
                        kb0, nb, tcol = chunks[ci]
                        W = nb * 128
                        k0 = kb0 * 128

                        def emit():
                            if light:
                                ins = nc.tensor.matmul(PS[pi][:, 0:W], lhsT=QT[s][:, k * 128:(k + 1) * 128], rhs=KT[s][:, k0:k0 + W],
                                                       start=True, stop=not is_fox)
                                if is_fox:
                                    ins = nc.tensor.matmul(PS[pi][:, 0:W], lhsT=sel[:, hd - 8, :], rhs=nchi[:, k0:k0 + W], start=False, stop=True)
                                return ins
                            nc.tensor.matmul(PS[pi][:, 0:W], lhsT=QT[s][:, k * 128:(k + 1) * 128], rhs=KT[s][:, k0:k0 + W], start=True, stop=False)
                            if is_fox:
                                hf = hd - 8
                                nc.tensor.matmul(PS[pi][:, 0:W], lhsT=sel[:, hf, :], rhs=nchi[:, k0:k0 + W], start=False, stop=False)
                                nc.tensor.matmul(PS[pi][:, 0:W], lhsT=sel[:, hf, :], rhs=ncmid[:, k0:k0 + W], start=False, stop=False)
                                last = (kb0 + nb - 1 != qb)
                                ins = nc.tensor.matmul(PS[pi][:, 0:W], lhsT=sel[:, hf, :], rhs=nclo[:, k0:k0 + W], start=False, stop=last)
                                if not last:
                                    ins = nc.tensor.matmul(PS[pi][:, 0:W], lhsT=ident_b[:], rhs=cm4[:], start=False, stop=True)
                            else:
                                need_val = (kb0 < 3)
                                nc.tensor.matmul(PS[pi][:, 0:W], lhsT=ident_b[:], rhs=Bhi[:, tcol:tcol + W], start=False, stop=False)
                                ins = nc.tensor.matmul(PS[pi][:, 0:W], lhsT=ident_b[:], rhs=Blo[:, tcol:tcol + W], start=False, stop=not need_val)
                                if need_val:
                                    ins = nc.tensor.matmul(PS[pi][:, 0:W], lhsT=ones_b[0:1, :], rhs=kval_b[0:1, k0:k0 + W], start=False, stop=True)
                            return ins
                        rd = [B_QT[s], B_KT[s]] + ([B_sel, B_ncs, B_ident_b, B_cm4] if is_fox else [B_ident_b, B_Bhi, B_Blo, B_ones_b, B_kvalb])
                        OP(pe, emit, reads=rd, writes=[PSB[pi]])
                        return W

                    for ci in range(nchk):
                        pi = next_ps(0, 2)
                        W = s_emit(pi, ci, light=True)
                        OP(dve, lambda pi=pi, ci=ci, W=W: nc.vector.reduce_max(out=mx[q2][:, ci:ci + 1], in_=PS[pi][:, 0:W], axis=AX.X),
                           reads=[PSB[pi]], writes=[B_mx[q2]])
                    OP(dve, lambda: nc.vector.reduce_max(out=mx[q2][:, 15:16], in_=mx[q2][:, 0:nchk], axis=AX.X), reads=[B_mx[q2]], writes=[B_mx[q2]])
                    OP(dve, lambda: nc.vector.tensor_scalar(out=mx[q2][:, 14:15], in0=mx[q2][:, 15:16], scalar1=-ISQ, scalar2=None, op0=ALU.mult),
                       reads=[B_mx[q2]], writes=[B_mx[q2]])
                    pcol = []
                    off = 0
                    for ci in range(nchk):
                        pi = next_ps(0, 2)
                        W = s_emit(pi, ci)
                        OP(act, lambda pi=pi, ci=ci, W=W, off=off: nc.scalar.activation(
                            out=Pm[q2][:, off:off + W], in_=PS[pi][:, 0:W], func=AF.Exp, bias=mx[q2][:, 14:15], scale=ISQ,
                            accum_out=rs[q2][:, ci:ci + 1]),
                           reads=[PSB[pi], B_mx[q2]], writes=[B_Pm[q2], B_rs[q2]])
                        pcol.append(off)
                        off += W
                    OP(dve, lambda: nc.vector.reduce_sum(out=rs[q2][:, 15:16], in_=rs[q2][:, 0:nchk], axis=AX.X), reads=[B_rs[q2]], writes=[B_rs[q2]])
                    OP(dve, lambda: nc.vector.reciprocal(out=rs[q2][:, 14:15], in_=rs[q2][:, 15:16]), reads=[B_rs[q2]], writes=[B_rs[q2]])
                    groups = []
                    for ci in range(nchk):
                        kb0, nb, _ = chunks[ci]
                        groups.append((kb0, nb, pcol[ci]))
                    po = 4
                    ngr = len(groups)
                    for gi, (kb0, nb, pc0) in enumerate(groups):
                        pti = ptc[0] % 4
                        ptc[0] += 1
                        pib = 5 + (pti % 2)
                        psv = PS[pib][:, :].bitcast(BF16)

                        def emit_t(nb=nb, pc0=pc0, psv=psv):
                            ins = None
                            for bb in range(nb):
                                ins = nc.tensor.transpose(out=psv[:, bb * 128:(bb + 1) * 128], in_=Pm[q2][:, pc0 + bb * 128:pc0 + (bb + 1) * 128],
                                                          identity=ident_b[:])
                            return ins
                        OP(pe, emit_t, reads=[B_Pm[q2], B_ident_b], writes=[PSB[pib]])
                        if gi % 2 == 0:
                            OP(dve, lambda nb=nb, pti=pti, psv=psv: nc.vector.tensor_copy(
                                out=PT[pti][:, 0:nb, :], in_=psv[:, 0:nb * 128].rearrange("p (a b) -> p a b", b=128)),
                               reads=[PSB[pib]], writes=[B_PT[pti]])
                        else:
                            OP(act, lambda nb=nb, pti=pti, psv=psv: nc.scalar.copy(
                                out=PT[pti][:, 0:nb, :], in_=psv[:, 0:nb * 128].rearrange("p (a b) -> p a b", b=128)),
                               reads=[PSB[pib]], writes=[B_PT[pti]])

                        def emit_pv(nb=nb, kb0=kb0, pti=pti, gi=gi):
                            ins = None
                            for bb in range(nb):
                                ins = nc.tensor.matmul(PS[po][:, 0:128], lhsT=PT[pti][:, bb, :], rhs=Vh[s][:, kb0 + bb, :],
                                                       start=(gi == 0 and bb == 0), stop=(gi == ngr - 1 and bb == nb - 1))
                            return ins
                        OP(pe, emit_pv, reads=[B_PT[pti], B_Vh[s]], writes=[PSB[po]])
                    OP(act, lambda: nc.scalar.activation(out=osb[q2][:], in_=PS[po][:, 0:128], func=AF.Copy, scale=rs[q2][:, 14:15]),
                       reads=[PSB[po], B_rs[q2]], writes=[B_osb[q2]])
                    psv7 = PS[7][:, :].bitcast(BF16)
                    OP(pe, lambda psv7=psv7: nc.tensor.transpose(out=psv7[:, 0:128], in_=osb[q2][:], identity=ident_b[:]),
                       reads=[B_osb[q2], B_ident_b], writes=[PSB[7]])
                    OP(dve, lambda psv7=psv7: nc.vector.tensor_copy(out=mixT[:, hd, k * 128:(k + 1) * 128], in_=psv7[:, 0:128]),
                       reads=[PSB[7]], writes=[B_mixT[hd]])
            B_MT = Buf()
            DMA(sp, MT_scr[:, :, :], mixT[:], reads=B_mixT, writes=[B_MT])
            if "mixT" in DBG:
                DMA(sp, DBG["mixT"].rearrange("p (c t) -> p c t", c=NCH), mixT[:], reads=B_mixT)
        ab.close()
        barrier()
        if STOP == "B":
            raise _Stop()

        with ExitStack() as st:
            acc = sb(st, "acc", [128, NT_OWN, D]); B_acc = [Buf() for _ in range(NT_OWN)]
            Gm = sb(st, "Gm", [128, D]); B_Gm = Buf()
            dg = [sb(st, f"dg{i}", [128, 128]) for i in range(2)]; B_dg = [Buf() for _ in range(2)]
            tmpc = sb(st, "tmpc", [128, 512]); B_tmpc = Buf()
            stats = sb(st, "stats", [128, 4, 6]); B_stats = Buf()
            mv = sb(st, "mv", [128, 4]); B_mv = Buf()

            def make_G(G, B_G, col0):
                for c in range(NCH):
                    d2 = c % 2
                    OP(dve, lambda c=c, d2=d2: nc.vector.tensor_scalar(out=dg[d2][:], in0=ident_f[:], scalar1=modc[:, col0 + c:col0 + c + 1],
                                                                         scalar2=None, op0=ALU.mult),
                       reads=[B_ident_f, B_modc], writes=[B_dg[d2]])
                    pi = 6 + (c % 2)
                    OP(pe, lambda d2=d2, pi=pi: nc.tensor.matmul(PS[pi][:, 0:128], lhsT=ones_f[:], rhs=dg[d2][:], start=True, stop=True),
                       reads=[B_ones_f, B_dg[d2]], writes=[PSB[pi]])
                    OP(dve, lambda c=c, pi=pi: nc.vector.tensor_copy(out=G[:, c * 128:(c + 1) * 128], in_=PS[pi][:, 0:128]),
                       reads=[PSB[pi]], writes=[B_G])

            def ln_inplace(ap, B_ap, gt, B_gt, bt, B_bt, dst=None, B_dst=None):
                if dst is None:
                    dst, B_dst = ap, B_ap
                for q in range(4):
                    OP(dve, lambda q=q: nc.vector.bn_stats(out=stats[:, q, :], in_=ap[:, q * 512:(q + 1) * 512]), reads=[B_ap], writes=[B_stats])
                OP(dve, lambda: nc.vector.bn_aggr(out=mv[:, 0:2], in_=stats[:, :, :].rearrange("p a b -> p (a b)")), reads=[B_stats], writes=[B_mv])
                OP(act, lambda: nc.scalar.activation(out=mv[:, 3:4], in_=mv[:, 1:2], func=AF.Sqrt, bias=eps_c[:, 0:1], scale=1.0),
                   reads=[B_mv, B_epsc], writes=[B_mv])
                OP(dve, lambda: nc.vector.reciprocal(out=mv[:, 2:3], in_=mv[:, 3:4]), reads=[B_mv], writes=[B_mv])
                OP(dve, lambda: nc.vector.tensor_scalar(out=dst, in0=ap, scalar1=mv[:, 0:1], scalar2=mv[:, 2:3], op0=ALU.subtract, op1=ALU.mult),
                   reads=[B_ap, B_mv], writes=[B_dst])
                OP(dve, lambda: nc.vector.tensor_tensor(out=dst, in0=dst, in1=gt[:], op=ALU.mult), reads=[B_dst, B_gt], writes=[B_dst])
                OP(dve, lambda: nc.vector.tensor_tensor(out=dst, in0=dst, in1=bt[:], op=ALU.add), reads=[B_dst, B_bt], writes=[B_dst])

            make_G(Gm, B_Gm, 80)
            with ExitStack() as st2:
                wo = sb(st2, "wo", [128, NCH, D], BF16); B_wo = Buf()
                Ga = sb(st2, "Ga", [128, D]); B_Ga = Buf()
                l1g = sb(st2, "l1g", [128, D]); B_l1g = Buf()
                l1b = sb(st2, "l1b", [128, D]); B_l1b = Buf()
                xo = sb(st2, "xo", [128, D]); B_xo = Buf()
                mixk = [sb(st2, f"mixk{i}", [128, NCH, 128], BF16) for i in range(2)]; B_mixk = [Buf() for _ in range(2)]
                w_out_v = w_out.rearrange("(k p) n -> p k n", p=128)
                for q in range(4):
                    DMA(pool, wo[:, :, q * 512:(q + 1) * 512], w_out_v[:, :, q * 512:(q + 1) * 512], writes=[B_wo])
                DMA(sp, l1g[:], ln1_g[0].partition_broadcast(128), writes=[B_l1g])
                DMA(sp, l1b[:], ln1_b[0].partition_broadcast(128), writes=[B_l1b])
                make_G(Ga, B_Ga, 32)
                for k in range(NT_OWN):
                    mk = k % 2
                    with nc.allow_non_contiguous_dma(reason="mixed tile reload, 256B rows"):
                        DMA(sp, mixk[mk][:], MT_scr[:, :, k * 128:(k + 1) * 128], reads=[B_MT], writes=[B_mixk[mk]])
                    DMA(sp, xo[:], xs[(4 * k + 3) * 128:(4 * k + 4) * 128, :], writes=[B_xo])
                    for nt in range(4):
                        pi = nt

                        def emit(nt=nt, pi=pi, mk=mk):
                            ins = None
                            for c in range(NCH):
                                ins = nc.tensor.matmul(PS[pi][:, :], lhsT=mixk[mk][:, c, :], rhs=wo[:, c, nt * 512:(nt + 1) * 512],
                                                       start=(c == 0), stop=(c == NCH - 1))
                            return ins
                        OP(pe, emit, reads=[B_mixk[mk], B_wo], writes=[PSB[pi]])
                        OP(dve, lambda nt=nt, pi=pi: nc.vector.tensor_tensor(out=tmpc[:], in0=PS[pi][:, :], in1=Ga[:, nt * 512:(nt + 1) * 512], op=ALU.mult),
                           reads=[PSB[pi], B_Ga], writes=[B_tmpc])
                        OP(dve, lambda nt=nt, k=k: nc.vector.scalar_tensor_tensor(out=acc[:, k, nt * 512:(nt + 1) * 512], in0=xo[:, nt * 512:(nt + 1) * 512],
                                                                                   scalar=ALPHA, in1=tmpc[:], op0=ALU.mult, op1=ALU.add),
                           reads=[B_xo, B_tmpc], writes=[B_acc[k]])
                    ln_inplace(acc[:, k, :], B_acc[k], l1g, B_l1g, l1b, B_l1b)
                    if "x1" in DBG:
                        DMA(sp, DBG["x1"][k * 128:(k + 1) * 128, :], acc[:, k, :], reads=[B_acc[k]])

            barrier()
            if STOP == "C1":
                raise _Stop()
            h2T = sb(st, "h2T", [128, NCH, 1024], BF16); B_h2T = Buf()
            gates = sb(st, "gates", [128, NT_OWN, E]); B_gates = Buf()
            bguc = sb(st, "bguc", [128, E, 32]); B_bguc = Buf()
            DMA(sp, bguc[:], b_gu_c[:, :, :], writes=[B_bguc])
            with ExitStack() as st2:
                h2f = sb(st2, "h2f", [128, NCH, 128]); B_h2f = Buf()
                wrt = sb(st2, "wrt", [128, NCH, E]); B_wrt = Buf()
                brt = sb(st2, "brt", [128, E]); B_brt = Buf()
                lg = sb(st2, "lg", [128, E]); B_lg = Buf()
                m8 = sb(st2, "m8", [128, 8]); B_m8 = Buf()
                eg = sb(st2, "eg", [128, E]); B_eg = Buf()
                msk = sb(st2, "msk", [128, E]); B_msk = Buf()
                sm = sb(st2, "sm", [128, 4]); B_sm = Buf()
                gT = sb(st2, "gT", [E, 128], BF16); B_gT = Buf()
                bdf = sb(st2, "bdf", [E_RUN, D]); B_bdf = Buf()
                bdb = sb(st2, "bdb", [E_RUN, D], BF16); B_bdb = Buf()
                with nc.allow_non_contiguous_dma(reason="router weights, 128B rows"):
                    DMA(sp, wrt[:], w_router.rearrange("(k p) e -> p k e", p=128), writes=[B_wrt])
                DMA(sp, brt[:], b_router[0].partition_broadcast(128), writes=[B_brt])
                DMA(sp, bdf[:], b_dn[:, :], writes=[B_bdf])
                OP(dve, lambda: nc.vector.tensor_copy(out=bdb[:], in_=bdf[:]), reads=[B_bdf], writes=[B_bdb])
                for k in range(NT_OWN):
                    x1 = acc[:, k, :]
                    B_x1 = B_acc[k]
                    for q4 in range(4):
                        pi = 4 + (q4 % 2)

                        def emit(q4=q4, pi=pi, x1=x1):
                            ins = None
                            for cc in range(4):
                                c = q4 * 4 + cc
                                ins = nc.tensor.transpose(out=PS[pi][:, cc * 128:(cc + 1) * 128], in_=x1[:, c * 128:(c + 1) * 128], identity=ident_f[:])
                            return ins
                        OP(pe, emit, reads=[B_x1, B_ident_f], writes=[PSB[pi]])
                        for cc in range(4):
                            c = q4 * 4 + cc
                            OP(act, lambda c=c, cc=cc, pi=pi, k=k: nc.scalar.activation(
                                out=h2T[:, c, k * 128:(k + 1) * 128], in_=PS[pi][:, cc * 128:(cc + 1) * 128], func=AF.Identity,
                                bias=modc[:, 48 + c:48 + c + 1], scale=sc1[:, 16 + c:16 + c + 1]),
                               reads=[PSB[pi], B_modc, B_sc1], writes=[B_h2T])
                            OP(dve, lambda c=c, cc=cc, pi=pi: nc.vector.tensor_scalar(
                                out=h2f[:, c, :], in0=PS[pi][:, cc * 128:(cc + 1) * 128], scalar1=sc1[:, 16 + c:16 + c + 1],
                                scalar2=modc[:, 48 + c:48 + c + 1], op0=ALU.mult, op1=ALU.add),
                               reads=[PSB[pi], B_modc, B_sc1], writes=[B_h2f])

                    def emit_r():
                        ins = None
                        for c in range(NCH):
                            ins = nc.tensor.matmul(PS[6][:, 0:E], lhsT=h2f[:, c, :], rhs=wrt[:, c, :], start=(c == 0), stop=(c == NCH - 1))
                        return ins
                    OP(pe, emit_r, reads=[B_h2f, B_wrt], writes=[PSB[6]])
                    OP(dve, lambda: nc.vector.tensor_tensor(out=lg[:], in0=PS[6][:, 0:E], in1=brt[:], op=ALU.add), reads=[PSB[6], B_brt], writes=[B_lg])
                    OP(dve, lambda: nc.vector.max(out=m8[:], in_=lg[:]), reads=[B_lg], writes=[B_m8])
                    OP(dve, lambda: nc.vector.tensor_scalar(out=msk[:], in0=lg[:], scalar1=m8[:, 3:4], scalar2=None, op0=ALU.is_ge),
                       reads=[B_lg, B_m8], writes=[B_msk])
                    OP(dve, lambda: nc.vector.tensor_scalar(out=sm[:, 0:1], in0=m8[:, 0:1], scalar1=-1.0, scalar2=None, op0=ALU.mult),
                       reads=[B_m8], writes=[B_sm])
                    OP(act, lambda: nc.scalar.activation(out=eg[:], in_=lg[:], func=AF.Exp, bias=sm[:, 0:1], scale=1.0), reads=[B_lg, B_sm], writes=[B_eg])
                    OP(dve, lambda: nc.vector.tensor_tensor(out=eg[:], in0=eg[:], in1=msk[:], op=ALU.mult), reads=[B_eg, B_msk], writes=[B_eg])
                    OP(dve, lambda: nc.vector.reduce_sum(out=sm[:, 1:2], in_=eg[:], axis=AX.X), reads=[B_eg], writes=[B_sm])
                    OP(dve, lambda: nc.vector.reciprocal(out=sm[:, 2:3], in_=sm[:, 1:2]), reads=[B_sm], writes=[B_sm])
                    OP(dve, lambda k=k: nc.vector.tensor_scalar(out=gates[:, k, :], in0=eg[:], scalar1=sm[:, 2:3], scalar2=None, op0=ALU.mult),
                       reads=[B_eg, B_sm], writes=[B_gates])
                    if "gates" in DBG:
                        DMA(sp, DBG["gates"][k * 128:(k + 1) * 128, :], gates[:, k, :], reads=[B_gates])
                    OP(pe, lambda k=k: nc.tensor.transpose(out=PS[7][0:E, 0:128], in_=gates[:, k, :], identity=ident_f[:]),
                       reads=[B_gates, B_ident_f], writes=[PSB[7]])
                    OP(dve, lambda: nc.vector.tensor_copy(out=gT[:], in_=PS[7][0:E, 0:128]), reads=[PSB[7]], writes=[B_gT])
                    for nt in range(4):
                        pi = nt
                        OP(pe, lambda nt=nt, pi=pi: nc.tensor.matmul(PS[pi][:, :], lhsT=gT[0:E_RUN, :], rhs=bdb[:, nt * 512:(nt + 1) * 512], start=True, stop=True),
                           reads=[B_gT, B_bdb], writes=[PSB[pi]])
                        OP(dve, lambda nt=nt, pi=pi: nc.vector.tensor_tensor(out=tmpc[:], in0=PS[pi][:, :], in1=Gm[:, nt * 512:(nt + 1) * 512], op=ALU.mult),
                           reads=[PSB[pi], B_Gm], writes=[B_tmpc])
                        OP(dve, lambda nt=nt, x1=x1: nc.vector.scalar_tensor_tensor(out=x1[:, nt * 512:(nt + 1) * 512], in0=x1[:, nt * 512:(nt + 1) * 512],
                                                                                     scalar=ALPHA, in1=tmpc[:], op0=ALU.mult, op1=ALU.add),
                           reads=[B_x1, B_tmpc], writes=[B_x1])

            barrier()
            if STOP == "C2":
                raise _Stop()
            with ExitStack() as st2:
                actT = sb(st2, "actT", [128, NCH, 1024], BF16); B_actT = Buf()
                RING = 4
                wring = [sb(st2, f"wring{i}", [128, NCH, 256], BF16) for i in range(RING)]; B_wring = [Buf() for _ in range(RING)]
                gq = [sb(st2, f"gq{i}", [128, 512]) for i in range(2)]; B_gq = [Buf() for _ in range(2)]
                sg = [sb(st2, f"sg{i}", [128, 512]) for i in range(2)]; B_sg = [Buf() for _ in range(2)]
                uq = [sb(st2, f"uq{i}", [128, 512]) for i in range(2)]; B_uq = [Buf() for _ in range(2)]
                t2 = [sb(st2, f"t2{i}", [128, 256]) for i in range(2)]; B_t2 = [Buf() for _ in range(2)]

                wblocks = []
                for e in range(E_RUN):
                    gu_v = w_gu[e].rearrange("(k p) n -> p k n", p=128)
                    dn_v = w_dn[e].rearrange("(k p) n -> p k n", p=128)
                    for sbk in range(8):
                        wblocks.append(gu_v[:, :, sbk * 256:(sbk + 1) * 256])
                        wblocks.append(gu_v[:, :, D + sbk * 256:D + (sbk + 1) * 256])
                    for nb in range(8):
                        wblocks.append(dn_v[:, :, nb * 256:(nb + 1) * 256])
                nload = [0]
                ncons = [0]

                def prefetch():
                    while nload[0] < len(wblocks) and nload[0] < ncons[0] + RING:
                        i = nload[0]
                        DMA(pool, wring[i % RING][:], wblocks[i], writes=[B_wring[i % RING]])
                        nload[0] += 1

                prefetch()
                ev = [0]
                for e in range(E_RUN):
                    for sbk in range(8):
                        ig = ncons[0]
                        iu = ncons[0] + 1
                        sg_, su_ = ig % RING, iu % RING
                        for fc in range(2):
                            fb = sbk * 2 + fc
                            for th in range(2):
                                pp = ev[0] % 2
                                ev[0] += 1
                                pg, pu = 2 * pp, 2 * pp + 1

                                def emit(fc=fc, th=th, pg=pg, pu=pu, sg_=sg_, su_=su_):
                                    ins = None
                                    for c in range(NCH):
                                        nc.tensor.matmul(PS[pg][:, :], lhsT=wring[sg_][:, c, fc * 128:(fc + 1) * 128],
                                                         rhs=h2T[:, c, th * 512:(th + 1) * 512], start=(c == 0), stop=(c == NCH - 1))
                                    for c in range(NCH):
                                        ins = nc.tensor.matmul(PS[pu][:, :], lhsT=wring[su_][:, c, fc * 128:(fc + 1) * 128],
                                                               rhs=h2T[:, c, th * 512:(th + 1) * 512], start=(c == 0), stop=(c == NCH - 1))
                                    return ins
                                OP(pe, emit, reads=[B_wring[sg_], B_wring[su_], B_h2T], writes=[PSB[pg], PSB[pu]])
                                OP(dve, lambda pp=pp, pg=pg, fb=fb, e=e: nc.vector.tensor_scalar(
                                    out=gq[pp][:], in0=PS[pg][:, :], scalar1=bguc[:, e, fb:fb + 1], scalar2=7.0, op0=ALU.add, op1=ALU.min),
                                   reads=[PSB[pg], B_bguc], writes=[B_gq[pp]])
                                OP(act, lambda pp=pp: nc.scalar.activation(out=sg[pp][:], in_=gq[pp][:], func=AF.Sigmoid, scale=1.702),
                                   reads=[B_gq[pp]], writes=[B_sg[pp]])
                                OP(dve, lambda pp=pp, pu=pu, fb=fb, e=e: nc.vector.tensor_scalar(
                                    out=uq[pp][:], in0=PS[pu][:, :], scalar1=bguc[:, e, 16 + fb:16 + fb + 1], scalar2=7.0, op0=ALU.add, op1=ALU.min),
                                   reads=[PSB[pu], B_bguc], writes=[B_uq[pp]])
                                OP(dve, lambda pp=pp: nc.vector.tensor_scalar(out=uq[pp][:], in0=uq[pp][:], scalar1=-7.0, scalar2=1.0, op0=ALU.max, op1=ALU.add),
                                   reads=[B_uq[pp]], writes=[B_uq[pp]])
                                OP(dve, lambda pp=pp: nc.vector.tensor_tensor(out=gq[pp][:], in0=gq[pp][:], in1=sg[pp][:], op=ALU.mult),
                                   reads=[B_gq[pp], B_sg[pp]], writes=[B_gq[pp]])
                                OP(dve, lambda pp=pp, fb=fb, th=th: nc.vector.tensor_tensor(out=actT[:, fb, th * 512:(th + 1) * 512], in0=gq[pp][:], in1=uq[pp][:],
                                                                                              op=ALU.mult),
                                   reads=[B_gq[pp], B_uq[pp]], writes=[B_actT])
                        ncons[0] += 2
                        prefetch()
                    for nb in range(8):
                        idn = ncons[0]
                        sd = idn % RING
                        for tl in range(NT_OWN):
                            pi = 4 + (ev[0] % 4)
                            tq = ev[0] % 2
                            ev[0] += 1

                            def emit(tl=tl, pi=pi, sd=sd):
                                ins = None
                                for c in range(NCH):
                                    ins = nc.tensor.matmul(PS[pi][:, 0:256], lhsT=actT[:, c, tl * 128:(tl + 1) * 128], rhs=wring[sd][:, c, :],
                                                           start=(c == 0), stop=(c == NCH - 1))
                                return ins
                            OP(pe, emit, reads=[B_actT, B_wring[sd]], writes=[PSB[pi]])
                            OP(dve, lambda tl=tl, pi=pi, tq=tq, nb=nb, e=e: nc.vector.scalar_tensor_tensor(
                                out=t2[tq][:], in0=PS[pi][:, 0:256], scalar=gates[:, tl, e:e + 1], in1=Gm[:, nb * 256:(nb + 1) * 256],
                                op0=ALU.mult, op1=ALU.mult),
                               reads=[PSB[pi], B_gates, B_Gm], writes=[B_t2[tq]])
                            OP(dve, lambda tl=tl, tq=tq, nb=nb: nc.vector.tensor_tensor(
                                out=acc[:, tl, nb * 256:(nb + 1) * 256], in0=acc[:, tl, nb * 256:(nb + 1) * 256], in1=t2[tq][:], op=ALU.add),
                               reads=[B_t2[tq], B_acc[tl]], writes=[B_acc[tl]])
                        ncons[0] += 1
                        prefetch()

            barrier()
            if STOP == "D":
                raise _Stop()
            with ExitStack() as st2:
                l2g = sb(st2, "l2g", [128, D]); B_l2g = Buf()
                l2b = sb(st2, "l2b", [128, D]); B_l2b = Buf()
                ot = [sb(st2, f"ot{i}", [128, D]) for i in range(2)]; B_ot = [Buf() for _ in range(2)]
                stats = sb(st2, "stats2", [128, 4, 6]); B_stats = Buf()
                mv = sb(st2, "mv2", [128, 4]); B_mv = Buf()
                DMA(sp, l2g[:], ln2_g[0].partition_broadcast(128), writes=[B_l2g])
                DMA(sp, l2b[:], ln2_b[0].partition_broadcast(128), writes=[B_l2b])
                last = []
                for k in range(NT_OWN):
                    o2 = k % 2
                    src = acc[:, k, :]
                    dst = ot[o2][:]
                    for q in range(4):
                        OP(dve, lambda q=q, src=src: nc.vector.bn_stats(out=stats[:, q, :], in_=src[:, q * 512:(q + 1) * 512]), reads=[B_acc[k]], writes=[B_stats])
                    OP(dve, lambda: nc.vector.bn_aggr(out=mv[:, 0:2], in_=stats[:, :, :].rearrange("p a b -> p (a b)")), reads=[B_stats], writes=[B_mv])
                    OP(act, lambda: nc.scalar.activation(out=mv[:, 3:4], in_=mv[:, 1:2], func=AF.Sqrt, bias=eps_c[:, 0:1], scale=1.0),
                       reads=[B_mv, B_epsc], writes=[B_mv])
                    OP(dve, lambda: nc.vector.reciprocal(out=mv[:, 2:3], in_=mv[:, 3:4]), reads=[B_mv], writes=[B_mv])
                    OP(dve, lambda src=src, dst=dst: nc.vector.tensor_scalar(out=dst, in0=src, scalar1=mv[:, 0:1], scalar2=mv[:, 2:3], op0=ALU.subtract, op1=ALU.mult),
                       reads=[B_acc[k], B_mv], writes=[B_ot[o2]])
                    OP(dve, lambda dst=dst: nc.vector.tensor_tensor(out=dst, in0=dst, in1=l2g[:], op=ALU.mult), reads=[B_ot[o2], B_l2g], writes=[B_ot[o2]])
                    OP(dve, lambda dst=dst: nc.vector.tensor_tensor(out=dst, in0=dst, in1=l2b[:], op=ALU.add), reads=[B_ot[o2], B_l2b], writes=[B_ot[o2]])
                    last.append(DMA(sp, y[k * 128:(k + 1) * 128, :], ot[o2][:], reads=[B_ot[o2]]))
                sp.wait(*last)
    except _Stop:
        pass
    for i, sem in enumerate(sp.ring):
        if sp.rcnt[i]:
            sp.wait((sem, 16 * sp.rcnt[i], f"sp_r{i}"))
    return nc


def _consts():
    ql = np.arange(128)[:, None]
    col = np.arange(17 * 128)[None, :]
    i = col // 128
    kl = col % 128
    delta = (16 - i) * 128 + ql - kl
    mult = ((delta >= 0) & (delta <= 128)).astype(np.float32) \
        + ((delta >= 0) & (delta % 4 == 0) & (delta <= 512)).astype(np.float32) \
        + ((delta >= 0) & (delta % 16 == 0) & (delta <= 2048)).astype(np.float32)
    cm4 = np.zeros((128, 512), np.float32)
    kk = np.arange(128)[None, :]
    cm4[:, 384:512] = np.where(kk > ql, NEG, 0.0)
    return mult.astype(np.float32), delta.astype(np.float32), cm4


def make_in_maps(inputs):
    f = lambda a: np.ascontiguousarray(np.asarray(a, dtype=np.float32))
    x = f(inputs["x"])
    c = f(inputs["c"])
    mult, delta, cm4 = _consts()
    shared = {
        "w_ada": f(inputs["w_ada"][0]),
        "b_ada_c": f(np.asarray(inputs["b_ada"][0]).reshape(96, 128).T),
        "w_in": f(inputs["w_in"][0]),
        "bf_c": f(np.asarray(inputs["b_forget"][0]).reshape(8, 1)),
        "w_out": f(inputs["w_out"][0]),
        "ln1_g": f(inputs["ln1_g"]), "ln1_b": f(inputs["ln1_b"]),
        "w_router": f(inputs["w_router"][0]),
        "b_router": f(inputs["b_router"]),
        "w_gu": f(inputs["w_gate_up"][0][:E_RUN]),
        "b_gu_c": f(np.asarray(inputs["b_gate_up"][0]).reshape(E, 32, 128).transpose(2, 0, 1)),
        "w_dn": f(inputs["w_down"][0][:E_RUN]),
        "b_dn": f(inputs["b_down"][0][:E_RUN]),
        "ln2_g": f(inputs["ln2_g"]), "ln2_b": f(inputs["ln2_b"]),
        "ident": np.eye(128, dtype=np.float32),
        "dmult": mult, "ddelta": delta, "cm4": cm4,
    }
    maps = []
    for i in range(8):
        b, j = i // 4, i % 4
        xs = np.zeros((S_LOC, D), np.float32)
        g0 = 128 * (j - 3)
        lo = max(0, -g0)
        xs[lo:S_LOC] = x[b, g0 + lo:g0 + S_LOC]
        kv = np.zeros((1, S_LOC), np.float32)
        kv[0, :lo] = NEG
        m = dict(shared)
        m["xs"] = xs
        m["kval"] = kv
        m["cb"] = f(c[b].reshape(NCH, 128).T)
        maps.append(m)
    return maps


def kernel(**inputs):
    nc = build_nc()
    maps = make_in_maps(inputs)
    res = run_bass_kernel_spmd(nc, maps, core_ids=list(range(8)))
    out = np.zeros((2, 4096, D), np.float32)
    for i in range(8):
        b, j = i // 4, i % 4
        yi = np.asarray(res.results[i]["y"]).reshape(NT_OWN, 128, D)
        for k in range(NT_OWN):
            g = (4 * k + j) * 128
            out[b, g:g + 128] = yi[k]
    return out
```
